# Optimizing a Trainium2 kernel written in Bass

```python
import jax, jax.numpy as jnp
from jax import lax
import numpy as np

D_MODEL = 4096
BATCH = 1
SEQ = 8192
DEPTH = 1

D_MIX = D_MODEL
CONV_CH = D_MIX // 4
ATTN_WIDTH = D_MIX - CONV_CH
HEAD_DIM = 128
N_HEADS = ATTN_WIDTH // HEAD_DIM
CONV_WIDTH = 3
IN_WIDTH = 3 * CONV_CH + 3 * ATTN_WIDTH
PATTERNS = ((128, 1), (512, 4), (2048, 16))
BAND = 128
ALIBI_MAX_EXP = 8.0
N_GROUPS = 8
EXPERTS_PER_GROUP = 8
N_EXPERTS = N_GROUPS * EXPERTS_PER_GROUP
TOP_K = 2
D_EXPERT = D_MODEL // 8
MOE_BLOCK = 128
EPS = 1e-6

kernel_name = "hybrid_conv_dilated_attn_hiermoe_adaln"


def rms_norm(x, g):
    xf = x.astype(jnp.float32)
    y = xf * lax.rsqrt(jnp.mean(xf * xf, axis=-1, keepdims=True) + EPS)
    return (y * g.astype(jnp.float32)).astype(x.dtype)


def causal_dwconv(u, w):
    ch = u.shape[-1]
    return lax.conv_general_dilated(
        u, w[:, None, :], window_strides=(1,), padding=[(CONV_WIDTH - 1, 0)],
        dimension_numbers=('NWC', 'WIO', 'NWC'), feature_group_count=ch)


def dilated_branch(q, k, v, slopes, window, dil):
    B, S, H, E = q.shape
    chunk = dil * BAND
    s_pad = -(-S // chunk) * chunk
    nb = s_pad // chunk

    def blocks(a):
        a = jnp.pad(a, ((0, 0), (0, s_pad - S), (0, 0), (0, 0)))
        return a.reshape(B, nb, BAND, dil, H, E)

    def with_prev(a):
        prev = jnp.pad(a, ((0, 0), (1, 0), (0, 0), (0, 0), (0, 0), (0, 0)))[:, :-1]
        return jnp.concatenate([prev, a], axis=2)

    qb = blocks(q)
    kb = with_prev(blocks(k))
    vb = with_prev(blocks(v))
    scores = jnp.einsum('bnirhe,bnurhe->bnrhiu', qb, kb)
    i = jnp.arange(BAND)[:, None]
    u = jnp.arange(2 * BAND)[None, :]
    steps = BAND + i - u
    n = jnp.arange(nb)[:, None, None]
    valid = (steps >= 0) & (steps <= window // dil) & ((n > 0) | (u >= BAND))
    alibi = -slopes[:, None, None] * (steps * dil).astype(jnp.float32)
    scores = jnp.where(valid[None, :, None, None], scores + alibi, -jnp.inf)
    m = scores.max(-1)
    p = jnp.exp(scores - m[..., None])
    s = p.sum(-1)
    o = jnp.einsum('bnrhiu,bnurhe->bnirhe', p, vb).reshape(B, s_pad, H, E)[:, :S]

    def to_seq(a):
        return a.transpose(0, 1, 4, 2, 3).reshape(B, s_pad, H)[:, :S]

    return to_seq(m), to_seq(s), o


def dilated_mixture_attention(q, k, v):
    H = q.shape[2]
    slopes = jnp.exp2(-ALIBI_MAX_EXP * jnp.arange(1, H + 1, dtype=jnp.float32) / H)
    parts = [dilated_branch(q, k, v, slopes, w, d) for (w, d) in PATTERNS]
    m_all = jnp.stack([p[0] for p in parts])
    s_all = jnp.stack([p[1] for p in parts])
    o_all = jnp.stack([p[2] for p in parts])
    wts = jnp.exp(m_all - m_all.max(0))
    den = (wts * s_all).sum(0)
    return (wts[..., None] * o_all).sum(0) / den[..., None]


def token_mixer(h, w_in, conv_w, q_norm_g, k_norm_g, g_branch, w_out):
    B, S, _ = h.shape
    z = h @ w_in
    c1, c2, c3 = CONV_CH, 2 * CONV_CH, 3 * CONV_CH
    b_gate, c_gate, v_conv, q, k, v = jnp.split(
        z, [c1, c2, c3, c3 + ATTN_WIDTH, c3 + 2 * ATTN_WIDTH], axis=-1)
    y_conv = b_gate * causal_dwconv(c_gate * v_conv, conv_w)
    q = rms_norm(q.reshape(B, S, N_HEADS, HEAD_DIM), q_norm_g).astype(jnp.float32) * (HEAD_DIM ** -0.5)
    k = rms_norm(k.reshape(B, S, N_HEADS, HEAD_DIM), k_norm_g).astype(jnp.float32)
    v = v.reshape(B, S, N_HEADS, HEAD_DIM).astype(jnp.float32)
    y_attn = dilated_mixture_attention(q, k, v).astype(h.dtype).reshape(B, S, ATTN_WIDTH)
    y = jnp.concatenate([rms_norm(y_conv, g_branch[:CONV_CH]),
                         rms_norm(y_attn, g_branch[CONV_CH:])], axis=-1)
    return y @ w_out


def hier_moe(h, w_group, w_expert, w1, w3, w2):
    B, S, D = h.shape
    T = B * S
    xt = h.reshape(T, D)
    g_logits = (xt @ w_group).astype(jnp.float32)
    g_prob = jax.nn.softmax(g_logits, axis=-1)
    g_sel = jnp.argmax(g_logits, axis=-1)
    g_w = jnp.take_along_axis(g_prob, g_sel[:, None], axis=-1)[:, 0]
    e_logits = (xt @ w_expert).astype(jnp.float32).reshape(T, N_GROUPS, EXPERTS_PER_GROUP)
    e_in = jnp.take_along_axis(e_logits, g_sel[:, None, None], axis=1)[:, 0]
    e_prob = jax.nn.softmax(e_in, axis=-1)
    top_w, top_i = lax.top_k(e_prob, TOP_K)
    top_w = top_w / top_w.sum(-1, keepdims=True) * g_w[:, None]
    expert_id = g_sel[:, None] * EXPERTS_PER_GROUP + top_i
    A = T * TOP_K
    flat_e = expert_id.reshape(A)
    flat_t = jnp.repeat(jnp.arange(T, dtype=jnp.int32), TOP_K)
    flat_w = top_w.reshape(A)
    order = jnp.argsort(flat_e)
    se, st, sw = flat_e[order], flat_t[order], flat_w[order]
    counts = jnp.bincount(flat_e, length=N_EXPERTS)
    starts = jnp.cumsum(counts) - counts
    pcounts = (counts + MOE_BLOCK - 1) // MOE_BLOCK * MOE_BLOCK
    pends = jnp.cumsum(pcounts)
    pstarts = pends - pcounts
    dest = pstarts[se] + jnp.arange(A) - starts[se]
    R = A + N_EXPERTS * MOE_BLOCK
    n_blk = R // MOE_BLOCK
    row_tok = jnp.full((R,), T, jnp.int32).at[dest].set(st)
    row_w = jnp.zeros((R,), jnp.float32).at[dest].set(sw)
    blk_e = jnp.minimum(jnp.searchsorted(pends, jnp.arange(n_blk) * MOE_BLOCK, side='right'),
                        N_EXPERTS - 1)
    x_pad = jnp.concatenate([xt, jnp.zeros((1, D), xt.dtype)], axis=0)
    xs = x_pad[row_tok].reshape(n_blk, MOE_BLOCK, D)

    def expert_block(args):
        xb, e = args
        return (jax.nn.silu(xb @ w1[e]) * (xb @ w3[e])) @ w2[e]

    ys = lax.map(expert_block, (xs, blk_e)).reshape(R, D)
    out = jnp.zeros((T + 1, D), jnp.float32).at[row_tok].add(
        ys.astype(jnp.float32) * row_w[:, None])[:T]
    return out.astype(h.dtype).reshape(B, S, D)


def hybrid_layer(x, c, w_ada, b_ada, g_mix, g_ffn, w_in, conv_w, q_norm_g, k_norm_g,
                 g_branch, w_out, w_group, w_expert, w1, w3, w2):
    mod = (jax.nn.silu(c) @ w_ada + b_ada)[:, None, :]
    sh1, sc1, gt1, sh2, sc2, gt2 = jnp.split(mod, 6, axis=-1)
    h = rms_norm(x, g_mix) * (1.0 + sc1) + sh1
    x = x + gt1 * token_mixer(h, w_in, conv_w, q_norm_g, k_norm_g, g_branch, w_out)
    h = rms_norm(x, g_ffn) * (1.0 + sc2) + sh2
    x = x + gt2 * hier_moe(h, w_group, w_expert, w1, w3, w2)
    return x


def setup_inputs(seed: int = 0) -> dict:
    key = jax.random.key(seed)
    ks = jax.random.split(key, 17)
    f32 = jnp.float32

    def nrm(k, shape, scale):
        return jax.random.normal(k, shape, f32) * scale

    L = DEPTH
    return {
        "x": nrm(ks[0], (BATCH, SEQ, D_MODEL), 1.0),
        "c": nrm(ks[1], (BATCH, D_MODEL), 1.0),
        "w_ada": nrm(ks[2], (L, D_MODEL, 6 * D_MODEL), 0.5 * D_MODEL ** -0.5),
        "b_ada": nrm(ks[3], (L, 6 * D_MODEL), 0.01),
        "g_mix": 1.0 + nrm(ks[4], (L, D_MODEL), 0.02),
        "g_ffn": 1.0 + nrm(ks[5], (L, D_MODEL), 0.02),
        "w_in": nrm(ks[6], (L, D_MODEL, IN_WIDTH), D_MODEL ** -0.5),
        "conv_w": nrm(ks[7], (L, CONV_WIDTH, CONV_CH), CONV_WIDTH ** -0.5),
        "q_norm_g": 1.0 + nrm(ks[8], (L, HEAD_DIM), 0.02),
        "k_norm_g": 1.0 + nrm(ks[9], (L, HEAD_DIM), 0.02),
        "g_branch": 1.0 + nrm(ks[10], (L, D_MIX), 0.02),
        "w_out": nrm(ks[11], (L, D_MIX, D_MODEL), D_MIX ** -0.5),
        "w_group": nrm(ks[12], (L, D_MODEL, N_GROUPS), D_MODEL ** -0.5),
        "w_expert": nrm(ks[13], (L, D_MODEL, N_EXPERTS), D_MODEL ** -0.5),
        "w1": nrm(ks[14], (L, N_EXPERTS, D_MODEL, D_EXPERT), D_MODEL ** -0.5),
        "w3": nrm(ks[15], (L, N_EXPERTS, D_MODEL, D_EXPERT), D_MODEL ** -0.5),
        "w2": nrm(ks[16], (L, N_EXPERTS, D_EXPERT, D_MODEL), D_EXPERT ** -0.5),
    }


def reference(x, c, w_ada, b_ada, g_mix, g_ffn, w_in, conv_w, q_norm_g, k_norm_g,
              g_branch, w_out, w_group, w_expert, w1, w3, w2):
    for l in range(DEPTH):
        x = hybrid_layer(x, c, w_ada[l], b_ada[l], g_mix[l], g_ffn[l], w_in[l], conv_w[l],
                         q_norm_g[l], k_norm_g[l], g_branch[l], w_out[l], w_group[l],
                         w_expert[l], w1[l], w3[l], w2[l])
    return x
```

```python
import numpy as np
from contextlib import ExitStack
import concourse.bass as bass
import concourse.mybir as mybir
from concourse.bass_utils import run_bass_kernel_spmd

F32 = mybir.dt.float32
BF16 = mybir.dt.bfloat16
ALU = mybir.AluOpType
AF = mybir.ActivationFunctionType
AX = mybir.AxisListType

NCORES = 8
D = 4096
S = 8192
TOK = S // NCORES
HALO = 2048
WIN = TOK + HALO
NWT = WIN // 128
KC = D // 128
NH = 24
EPS = 1e-6
NEG = -30000.0
SOFF = 384
SLEN = SOFF + 2048 + 512
NEXP = 64
import os
LIM = int(os.environ.get('KLIM', '0'))
SKIP = os.environ.get('KSKIP', '')
LIMM = int(os.environ.get('KLIMM', '0'))


class Prog:
    NQ = 10

    def __init__(self, nc, es):
        self.nc = nc
        self.engs = {'pe': nc.tensor, 'dve': nc.vector, 'act': nc.scalar,
                     'pool': nc.gpsimd, 'sp': nc.sync}
        self.sems = []
        self.csem = {}
        self.ccnt = {}
        for e in ['pe', 'dve', 'act', 'pool']:
            self.csem[e] = self._newsem(es, 'c_' + e)
            self.ccnt[e] = 0
        self.dsem = {}
        self.dcnt = {}
        self.dnext = {}
        for q in ['sp', 'act', 'pool']:
            self.dsem[q] = [self._newsem(es, 'd_%s%d' % (q, i)) for i in range(self.NQ)]
            self.dcnt[q] = [0] * self.NQ
            self.dnext[q] = 0
        self.seen = {e: {} for e in self.engs}
        self.lastw = {}
        self.readers = {}
        self.nins = 0
        self.pe_pending_wait = False
        self.pe_dummy = None

    def _newsem(self, es, name):
        h = es.enter_context(self.nc.semaphore(name))
        self.sems.append(h)
        return len(self.sems) - 1

    def _wait(self, e, tok):
        si, val = tok
        if e == 'pe' and si == self.csem['pe']:
            return
        if self.seen[e].get(si, 0) >= val:
            return
        if e == 'pe' and self.pe_pending_wait and self.pe_dummy is not None:
            self.engs[e].ldweights(self.pe_dummy)
        self.engs[e].wait_ge(self.sems[si], val)
        if e == 'pe':
            self.pe_pending_wait = True
        self.seen[e][si] = val

    def _deps(self, reads, writes):
        deps = {}
        def add(tok):
            if tok is None:
                return
            si, v = tok
            if deps.get(si, 0) < v:
                deps[si] = v
        for k in reads:
            add(self.lastw.get(k))
        for k in writes:
            add(self.lastw.get(k))
            for t in self.readers.get(k, ()):
                add(t)
        return list(deps.items())

    def _record(self, tok, reads, writes):
        for k in reads:
            self.readers.setdefault(k, []).append(tok)
        for k in writes:
            self.lastw[k] = tok
            self.readers[k] = []

    def op(self, e, fn, reads=(), writes=()):
        for tok in self._deps(reads, writes):
            self._wait(e, tok)
        ins = fn(self.engs[e])
        if e == 'pe':
            self.pe_pending_wait = False
        self.ccnt[e] += 1
        ins.then_inc(self.sems[self.csem[e]], 1)
        self._record((self.csem[e], self.ccnt[e]), reads, writes)
        self.nins += 1

    def dma(self, q, out, in_, reads=(), writes=(), **kw):
        i = self.dnext[q]
        self.dnext[q] = (i + 1) % self.NQ
        si = self.dsem[q][i]
        if self.dcnt[q][i]:
            self._wait(q, (si, self.dcnt[q][i]))
        for tok in self._deps(reads, writes):
            self._wait(q, tok)
        ins = self.engs[q].dma_start(out=out, in_=in_, **kw)
        self.dcnt[q][i] += 16
        ins.then_inc(self.sems[si], 16)
        self._record((si, self.dcnt[q][i]), reads, writes)
        self.nins += 1

    def barrier(self):
        toks = []
        for e in self.csem:
            if self.ccnt[e]:
                toks.append((self.csem[e], self.ccnt[e]))
        for q in self.dsem:
            for i in range(self.NQ):
                if self.dcnt[q][i]:
                    toks.append((self.dsem[q][i], self.dcnt[q][i]))
        for e in self.engs:
            for t in toks:
                if e in self.csem and t[0] == self.csem[e]:
                    continue
                self._wait(e, t)
        self.lastw = {}
        self.readers = {}


def slopes():
    return [float(2.0 ** (-8.0 * (h + 1) / NH)) for h in range(NH)]


def build(stages=99, dbg=None):
    nc = bass.Bass("TRN2", target_bir_lowering=False)

    def din(name, shape, dt=F32):
        return nc.dram_tensor(name, list(shape), dt, kind="ExternalInput").ap()

    dbg = set(dbg or ())

    INSHAPES = {
        "xw": ([WIN, D], F32), "c_t": ([128, KC], F32), "w_ada": ([D, 6 * D], F32),
        "b_ada": ([1, 6 * D], F32), "g_mix_t": ([128, KC], F32), "g_ffn_t": ([128, KC], F32),
        "g_br_t": ([128, KC], F32), "w_in_t": ([96, 128, KC * 128], F32),
        "conv_w_t": ([128, 24], F32), "qg": ([128, 1], F32), "kg": ([128, 1], F32),
        "w_out": ([D, D], F32), "w_r": ([128, KC * 72], F32),
        "w1_t": ([NEXP, 4, 128, KC * 128], F32), "w3_t": ([NEXP, 4, 128, KC * 128], F32),
        "w2_t": ([NEXP, 8, 128, 4 * 512], F32), "ident": ([128, 128], F32),
        "tdelta": ([128, SLEN], F32), "tmult": ([128, SLEN], BF16),
        "keybias": ([128, NWT], F32), "cmask": ([128, 2], F32),
    }
    _in = {}

    def IN(name):
        if name not in _in:
            shp, dt = INSHAPES[name]
            _in[name] = din(name, shp, dt)
        return _in[name]

    out = nc.dram_tensor("out", [TOK, D], F32, kind="ExternalOutput").ap()
    dbg_t = None
    if dbg:
        dbg_t = nc.dram_tensor("dbg", [128, 256], F32, kind="ExternalOutput").ap()

    def dscr(name, shape, dt):
        kind = "ExternalOutput" if name in dbg else "Internal"
        return nc.dram_tensor(name, list(shape), dt, kind=kind).ap()

    hT_d = dscr("hT_d", [NWT, 128, KC * 128], BF16)
    yT_d = dscr("yT_d", [128, KC, TOK], BF16)
    gtb_d = dscr("gtb_d", [2, 128, D], F32)
    wgtT_d = dscr("wgtT_d", [NEXP, TOK], F32)

    es = ExitStack()
    with es:
        P = Prog(nc, es)

        def sb(name, shape, dt=F32, stack=es):
            return stack.enter_context(nc.sbuf_tensor(name, list(shape), dt))

        ps = [es.enter_context(nc.psum_tensor("ps%d" % i, [128, 512], F32)) for i in range(8)]

        ident = sb("ident_sb", [128, 128])
        ones_f = sb("ones_f", [128, 128])
        ones_b = sb("ones_b", [128, 128], BF16)
        modT = sb("modT", [128, 192])
        A1 = sb("A1", [128, KC])
        A2 = sb("A2", [128, KC])
        gbr = sb("gbr", [128, KC])
        tmp32 = sb("tmp32", [128, KC])
        rstd_c = sb("rstd_c", [128, 8])
        rstd_a = sb("rstd_a", [128, 8])

        P.dma('sp', ident[:, :], IN("ident")[:, :], writes=['ident'])
        P.op('dve', lambda e: e.memset(ones_f[:, :], 1.0), writes=['ones_f'])
        P.op('dve', lambda e: e.memset(ones_b[:, :], 1.0), writes=['ones_b'])
        P.dma('sp', gbr[:, :], IN("g_br_t")[:, :], writes=['gbr'])

        def B1(kc):
            return modT[:, 0 * KC + kc:0 * KC + kc + 1]

        def B2(kc):
            return modT[:, 3 * KC + kc:3 * KC + kc + 1]

        with ExitStack() as st:
            c_sb = sb("c_sb", [128, KC], stack=st)
            sc = sb("sc", [128, KC], stack=st)
            wa = [sb("wa%d" % i, [128, 2048], stack=st) for i in range(3)]
            brow = sb("brow", [1, 2048], stack=st)
            row = sb("row", [1, 2048], stack=st)
            gstage = sb("gstage", [128, 2048], stack=st)
            gm = sb("gm", [128, KC], stack=st)
            gf = sb("gf", [128, KC], stack=st)

            P.dma('sp', c_sb[:, :], IN("c_t")[:, :], writes=['c_sb'])
            P.dma('sp', gm[:, :], IN("g_mix_t")[:, :], writes=['gm'])
            P.dma('sp', gf[:, :], IN("g_ffn_t")[:, :], writes=['gf'])
            P.op('act', lambda e: e.activation(out=sc[:, :], in_=c_sb[:, :], func=AF.Silu),
                 reads=['c_sb'], writes=['sc'])
            nd = 0
            for cb2 in (range(12) if not LIMM else (0, 4)):
                c0 = cb2 * 2048
                P.dma('sp', brow[0:1, :], IN("b_ada")[0:1, c0:c0 + 2048], writes=['brow'])
                for kc in range(KC):
                    buf = nd % 3
                    nd += 1
                    P.dma('sp', wa[buf][:, :], IN("w_ada")[kc * 128:(kc + 1) * 128, c0:c0 + 2048],
                          writes=['wa%d' % buf])

                    def mm(e, kc=kc, buf=buf):
                        for n in range(4):
                            r = e.matmul(ps[n][0:1, :], lhsT=sc[:, kc:kc + 1],
                                         rhs=wa[buf][:, n * 512:(n + 1) * 512],
                                         start=(kc == 0), stop=(kc == KC - 1))
                        return r
                    P.op('pe', mm, reads=['sc', 'wa%d' % buf], writes=['psM'])
                for n in range(4):
                    P.op('dve', lambda e, n=n: e.tensor_tensor(
                        out=row[0:1, n * 512:(n + 1) * 512], in0=ps[n][0:1, :],
                        in1=brow[0:1, n * 512:(n + 1) * 512], op=ALU.add),
                        reads=['psM', 'brow'], writes=['row'])

                def tr(e, cb2=cb2):
                    for q in range(16):
                        j = cb2 * 16 + q
                        r = e.matmul(ps[4][:, j:j + 1], lhsT=row[0:1, q * 128:(q + 1) * 128],
                                     rhs=ones_f[0:1, 0:1], start=True, stop=True)
                    return r
                P.op('pe', tr, reads=['row', 'ones_f'], writes=['psT'])
                if cb2 in (4, 5, 10, 11):
                    which = 0 if cb2 < 6 else 1
                    gc0 = (cb2 % 2) * 2048

                    def bc(e):
                        for n in range(4):
                            r = e.matmul(ps[n][:, :], lhsT=ones_f[0:1, :],
                                         rhs=row[0:1, n * 512:(n + 1) * 512], start=True, stop=True)
                        return r
                    P.op('pe', bc, reads=['row', 'ones_f'], writes=['psM'])
                    for n in range(4):
                        P.op('act', lambda e, n=n: e.copy(out=gstage[:, n * 512:(n + 1) * 512],
                                                          in_=ps[n][:, :]),
                             reads=['psM'], writes=['gstage'])
                    P.dma('sp', gtb_d[which, :, gc0:gc0 + 2048], gstage[:, :],
                          reads=['gstage'], writes=['gtb_d'])
            if LIMM:
                P.op('dve', lambda e: e.memset(ps[4][:, 0:192], 0.5), writes=['psT'])
            P.op('dve', lambda e: e.tensor_copy(out=modT[:, :], in_=ps[4][:, 0:192]),
                 reads=['psT'], writes=['modT'])
            P.op('dve', lambda e: e.tensor_scalar(out=tmp32[:, :], in0=modT[:, KC:2 * KC], scalar1=1.0,
                                                  scalar2=None, op0=ALU.add),
                 reads=['modT'], writes=['tmp32'])
            P.op('dve', lambda e: e.tensor_tensor(out=A1[:, :], in0=tmp32[:, :], in1=gm[:, :], op=ALU.mult),
                 reads=['tmp32', 'gm'], writes=['A1'])
            P.op('dve', lambda e: e.tensor_scalar(out=tmp32[:, :], in0=modT[:, 4 * KC:5 * KC], scalar1=1.0,
                                                  scalar2=None, op0=ALU.add),
                 reads=['modT', 'A1'], writes=['tmp32'])
            P.op('dve', lambda e: e.tensor_tensor(out=A2[:, :], in0=tmp32[:, :], in1=gf[:, :], op=ALU.mult),
                 reads=['tmp32', 'gf'], writes=['A2'])
            P.barrier()

        if dbg:
            P.dma('sp', dbg_t[:, 0:192], modT[:, :], reads=['modT'], writes=['dbg'])
            P.dma('sp', dbg_t[:, 192:224], A1[:, :], reads=['A1'], writes=['dbg'])
            P.dma('sp', dbg_t[:, 224:256], A2[:, :], reads=['A2'], writes=['dbg'])

        def norm_transpose(src_ap, xt, junk, ss, rs, hTdst, Aap, Bfn, key, pb, hTf=None):
            P.dma('sp', xt[:, :], src_ap, reads=[key + 'src'], writes=[key + 'xt'])
            P.op('dve', lambda e: e.memset(ss[:, :], 0.0), writes=[key + 'ss'])
            P.op('act', lambda e: e.activation(out=junk[:, :], in_=xt[:, :], func=AF.Square,
                                               accum_out=ss[:, 0:1]),
                 reads=[key + 'xt', key + 'ss'], writes=[key + 'junk', key + 'ss'])
            P.op('act', lambda e: e.activation(out=rs[:, 0:1], in_=ss[:, 0:1], func=AF.Sqrt,
                                               scale=1.0 / D, bias=EPS),
                 reads=[key + 'ss'], writes=[key + 'rs'])
            P.op('dve', lambda e: e.reciprocal(out=rs[:, 1:2], in_=rs[:, 0:1]),
                 reads=[key + 'rs'], writes=[key + 'rstd'])
            P.op('dve', lambda e: e.tensor_scalar(out=xt[:, :], in0=xt[:, :], scalar1=rs[:, 1:2],
                                                  scalar2=None, op0=ALU.mult),
                 reads=[key + 'xt', key + 'rstd'], writes=[key + 'xt'])
            for k4 in range(KC // 4 if 'tp' not in SKIP else 0):
                bank = pb[k4 % 2]
                bk = key + 'pst%d' % (k4 % 2)

                def tp(e, k4=k4, bank=bank):
                    for q in range(4):
                        kc = k4 * 4 + q
                        r = e.transpose(out=ps[bank][:, q * 128:(q + 1) * 128],
                                        in_=xt[:, kc * 128:(kc + 1) * 128], identity=ident[:, :])
                    return r
                P.op('pe', tp, reads=[key + 'xt', 'ident'], writes=[bk])
                for q in range(4 if 'evac' not in SKIP else 0):
                    kc = k4 * 4 + q
                    hk = key + 'hT%d' % kc
                    if hTf is not None:
                        P.op('act', lambda e, kc=kc, q=q, bank=bank: e.activation(
                            out=hTf[:, kc, :], in_=ps[bank][:, q * 128:(q + 1) * 128], func=AF.Identity,
                            scale=Aap[:, kc:kc + 1], bias=Bfn(kc)),
                            reads=[bk, 'A1', 'A2', 'modT'], writes=[hk + 'f'])
                        P.op('pool', lambda e, kc=kc: e.tensor_copy(out=hTdst(kc), in_=hTf[:, kc, :]),
                             reads=[hk + 'f'], writes=[hk])
                    elif k4 % 2 == 0:
                        P.op('dve', lambda e, kc=kc, q=q, bank=bank: e.tensor_scalar(
                            out=hTdst(kc), in0=ps[bank][:, q * 128:(q + 1) * 128],
                            scalar1=Aap[:, kc:kc + 1], scalar2=Bfn(kc), op0=ALU.mult, op1=ALU.add),
                            reads=[bk, 'A1', 'A2', 'modT'], writes=[hk])
                    else:
                        P.op('act', lambda e, kc=kc, q=q, bank=bank: e.activation(
                            out=hTdst(kc), in_=ps[bank][:, q * 128:(q + 1) * 128], func=AF.Identity,
                            scale=Aap[:, kc:kc + 1], bias=Bfn(kc)),
                            reads=[bk, 'A1', 'A2', 'modT'], writes=[hk])

        if stages >= 2:
            with ExitStack() as st:
                xts = [sb("xt%d" % i, [128, D], stack=st) for i in range(2)]
                junk = sb("junk", [128, D], BF16, stack=st)
                sss = [sb("ss%d" % i, [128, 1], stack=st) for i in range(2)]
                rss = [sb("rs%d" % i, [128, 2], stack=st) for i in range(2)]
                hTs = [sb("hTs%d" % i, [128, KC, 128], BF16, stack=st) for i in range(2)]
                for t in range(NWT):
                    b = t % 2
                    key = 's1_%d_' % b
                    norm_transpose(IN("xw")[t * 128:(t + 1) * 128, :], xts[b], junk, sss[b], rss[b],
                                   (lambda kc, b=b: hTs[b][:, kc, :]), A1, B1, key, (0, 1))
                    if 'store' in SKIP:
                        continue
                    P.dma('sp', hT_d[t, :, :], hTs[b][:, :, :].rearrange('p k t -> p (k t)'),
                          reads=[key + 'hT%d' % kc for kc in range(KC)], writes=['hT_d%d' % t])
                P.barrier()

        def W_IN(cb):
            return IN("w_in_t")[cb, :, :]

        cnt = {'l': 0, 'h': 0, 'b': 0, 'k': 0, 's': 0, 'y': 0}

        if stages >= 3:
            with ExitStack() as st:
                stg = [sb("wstg%d" % i, [128, KC * 128], stack=st) for i in range(2)]
                Wq = sb("Wq", [128, 2, KC, 128], BF16, stack=st)
                Wk = sb("Wk", [128, 2, KC, 128], BF16, stack=st)
                Wv = sb("Wv", [128, KC, 2, 128], BF16, stack=st)
                hTc = [sb("hTc%d" % i, [128, 2, KC, 128], BF16, stack=st) for i in range(2)]
                ssq_c = sb("ssq_c", [128, TOK], stack=st)
                ssq_a = sb("ssq_a", [128, TOK], stack=st)
                row1 = sb("row1", [1, TOK], stack=st)

                def load_block(cb, dst3, dkey):
                    i = cnt['l'] % 2
                    cnt['l'] += 1
                    P.dma('sp', stg[i][:, :], W_IN(cb), writes=['stg%d' % i])
                    src3 = stg[i][:, :].rearrange('p (k c) -> p k c', k=KC)
                    if i == 0:
                        P.op('pool', lambda e: e.tensor_copy(out=dst3, in_=src3), reads=['stg%d' % i], writes=[dkey])
                    else:
                        P.op('act', lambda e: e.copy(out=dst3, in_=src3), reads=['stg%d' % i], writes=[dkey])

                def load_hT(t0):
                    hb = cnt['h'] % 2
                    cnt['h'] += 1
                    P.dma('sp', hTc[hb][:, :, :, :].rearrange('p t k c -> p t (k c)'),
                          hT_d[t0:t0 + 2, :, :].rearrange('t p f -> p t f'), writes=['hTc%d' % hb])
                    return hb

                def proj_fm(wsrc, wkey, hb, bank, bkey):
                    def mm(e):
                        for kc in range(KC):
                            r = e.matmul(ps[bank][:, 0:256], lhsT=wsrc[:, kc, :], rhs=hTc[hb][:, :, kc, :],
                                         start=(kc == 0), stop=(kc == KC - 1))
                        return r
                    P.op('pe', mm, reads=[wkey, 'hTc%d' % hb], writes=[bkey])

                def finish_rstd(ssq, nfeat, dst, key):
                    P.op('dve', lambda e: e.tensor_scalar(out=row1[0:1, :], in0=ssq[0:1, :], scalar1=1.0 / nfeat,
                                                          scalar2=EPS, op0=ALU.mult, op1=ALU.add),
                         reads=[key], writes=['row1'])
                    P.op('act', lambda e: e.activation(out=row1[0:1, :], in_=row1[0:1, :], func=AF.Sqrt),
                         reads=['row1'], writes=['row1'])
                    P.op('dve', lambda e: e.reciprocal(out=row1[0:1, :], in_=row1[0:1, :]),
                         reads=['row1'], writes=['row1'])

                    def tr(e):
                        for t in range(8):
                            r = e.matmul(ps[2][:, t:t + 1], lhsT=row1[0:1, t * 128:(t + 1) * 128],
                                         rhs=ones_f[0:1, 0:1], start=True, stop=True)
                        return r
                    P.op('pe', tr, reads=['row1', 'ones_f'], writes=['pb2'])
                    P.op('dve', lambda e: e.tensor_copy(out=dst[:, :], in_=ps[2][:, 0:8]),
                         reads=['pb2'], writes=[key + 'rstd'])

                if LIM:
                    zt = sb('zt', [128, TOK], BF16, stack=st)
                    P.op('dve', lambda e: e.memset(zt[:, :], 0.0), writes=['zt'])
                    for k in range(KC):
                        P.dma('sp', yT_d[:, k, :], zt[:, :], reads=['zt'], writes=['yT_d%d' % k, 'yT_d%d_0' % k, 'yT_d%d_1' % k])
                with ExitStack() as s2:
                    u = sb("u", [128, 1280], stack=s2)
                    Bsb = sb("Bsb", [128, 1280], stack=s2)
                    Csb = sb("Csb", [128, 256], stack=s2)
                    tcv = sb("tcv", [128, TOK], stack=s2)
                    yc = sb("yc", [128, TOK], stack=s2)
                    ycsq = sb("ycsq", [128, TOK], BF16, stack=s2)
                    ycg = sb("ycg", [128, TOK], BF16, stack=s2)
                    cw = sb("cw", [128, 24], stack=s2)
                    cmask = sb("cmask_sb", [128, 2], stack=s2)
                    P.dma('sp', cw[:, :], IN("conv_w_t")[:, :], writes=['cw'])
                    P.dma('sp', cmask[:, :], IN("cmask")[:, :], writes=['cmask'])
                    P.op('dve', lambda e: e.memset(ssq_c[:, :], 0.0), writes=['ssq_c'])
                    for cblk in range(8 if not LIM else 1):
                        load_block(cblk, Wq[:, 0, :, :], 'Wq0')
                        load_block(8 + cblk, Wq[:, 1, :, :], 'Wq1')
                        load_block(16 + cblk, Wk[:, 0, :, :], 'Wk0')
                        for ch in range(5):
                            hb = load_hT(14 + 2 * ch)
                            c0 = ch * 256
                            proj_fm(Wq[:, 0, :, :], 'Wq0', hb, 0, 'pb0')
                            proj_fm(Wq[:, 1, :, :], 'Wq1', hb, 1, 'pb1')
                            proj_fm(Wk[:, 0, :, :], 'Wk0', hb, 3, 'pb3')
                            P.op('act', lambda e, c0=c0: e.copy(out=Bsb[:, c0:c0 + 256], in_=ps[0][:, 0:256]),
                                 reads=['pb0'], writes=['Bsb'])
                            P.op('act', lambda e: e.copy(out=Csb[:, :], in_=ps[1][:, 0:256]),
                                 reads=['pb1'], writes=['Csb'])
                            P.op('dve', lambda e, c0=c0: e.tensor_tensor(out=u[:, c0:c0 + 256], in0=ps[3][:, 0:256],
                                                                         in1=Csb[:, :], op=ALU.mult),
                                 reads=['pb3', 'Csb'], writes=['u'])
                        P.op('dve', lambda e: e.tensor_tensor(out=u[:, 254:256], in0=u[:, 254:256], in1=cmask[:, :],
                                                              op=ALU.mult), reads=['u', 'cmask'], writes=['u'])

                        def cwc(j, cblk=cblk):
                            return cw[:, cblk * 3 + j:cblk * 3 + j + 1]
                        P.op('dve', lambda e: e.tensor_scalar(out=tcv[:, :], in0=u[:, 256:1280], scalar1=cwc(2),
                                                              scalar2=None, op0=ALU.mult),
                             reads=['u', 'cw'], writes=['tcv'])
                        P.op('dve', lambda e: e.scalar_tensor_tensor(out=tcv[:, :], in0=u[:, 255:1279], scalar=cwc(1),
                                                                     in1=tcv[:, :], op0=ALU.mult, op1=ALU.add),
                             reads=['u', 'cw', 'tcv'], writes=['tcv'])
                        P.op('dve', lambda e: e.scalar_tensor_tensor(out=tcv[:, :], in0=u[:, 254:1278], scalar=cwc(0),
                                                                     in1=tcv[:, :], op0=ALU.mult, op1=ALU.add),
                             reads=['u', 'cw', 'tcv'], writes=['tcv'])
                        P.op('dve', lambda e: e.tensor_tensor(out=yc[:, :], in0=Bsb[:, 256:1280], in1=tcv[:, :],
                                                              op=ALU.mult), reads=['Bsb', 'tcv'], writes=['yc'])
                        P.op('pool', lambda e: e.tensor_tensor(out=ycsq[:, :], in0=yc[:, :], in1=yc[:, :], op=ALU.mult),
                             reads=['yc'], writes=['ycsq'])
                        P.op('pool', lambda e, cblk=cblk: e.tensor_scalar(out=ycg[:, :], in0=yc[:, :],
                                                                          scalar1=gbr[:, cblk:cblk + 1], scalar2=None,
                                                                          op0=ALU.mult),
                             reads=['yc', 'gbr'], writes=['ycg'])
                        P.dma('sp', yT_d[:, cblk, :], ycg[:, :], reads=['ycg'], writes=['yT_d%d' % cblk])
                        for n in range(2):
                            P.op('pe', lambda e, n=n: e.matmul(ps[2][:, :], lhsT=ones_b[:, :],
                                                               rhs=ycsq[:, n * 512:(n + 1) * 512], start=True, stop=True),
                                 reads=['ycsq', 'ones_b'], writes=['pb2'])
                            P.op('dve', lambda e, n=n: e.tensor_tensor(out=ssq_c[:, n * 512:(n + 1) * 512],
                                                                       in0=ps[2][:, :], in1=ssq_c[:, n * 512:(n + 1) * 512],
                                                                       op=ALU.add),
                                 reads=['pb2', 'ssq_c'], writes=['ssq_c'])
                    finish_rstd(ssq_c, 1024.0, rstd_c, 'ssq_c')
                    P.barrier()

                if stages >= 4:
                    with ExitStack() as s3:
                        KT = sb("KT", [128, 2, WIN], BF16, stack=s3)
                        QT = sb("QT", [128, 2, TOK], BF16, stack=s3)
                        Vsb = sb("Vsb", [128, NWT, 256], BF16, stack=s3)
                        tdel = sb("tdel", [128, SLEN], stack=s3)
                        tmul = sb("tmul", [128, SLEN], BF16, stack=s3)
                        kbias = sb("kbias", [128, NWT], stack=s3)
                        kfb = [sb("kfb%d" % i, [128, 256], stack=s3) for i in range(2)]
                        sqb = [sb("sqb%d" % i, [128, 256], BF16, stack=s3) for i in range(2)]
                        v1 = sb("v1", [128, 256], stack=s3)
                        v2 = sb("v2", [128, 256], stack=s3)
                        v3 = sb("v3", [128, 256], stack=s3)
                        ssb = [sb("ssb%d" % i, [128, 512], stack=s3) for i in range(4)]
                        eb = [sb("eb%d" % i, [128, 512], BF16, stack=s3) for i in range(4)]
                        pTb = [sb("pTb%d" % i, [128, 512], BF16, stack=s3) for i in range(4)]
                        rec = sb("rec", [128, 512], stack=s3)
                        yaf = sb("yaf", [128, 512], stack=s3)
                        ysq = sb("ysq", [128, 512], BF16, stack=s3)
                        yst = [sb("yst%d" % i, [128, 512], BF16, stack=s3) for i in range(2)]
                        qgs = sb("qgs", [128, 1], stack=s3)
                        kgs = sb("kgs", [128, 1], stack=s3)
                        P.dma('sp', tdel[:, :], IN("tdelta")[:, :], writes=['tdel'])
                        P.dma('sp', tmul[:, :], IN("tmult")[:, :], writes=['tmul'])
                        P.dma('sp', kbias[:, :], IN("keybias")[:, :], writes=['kbias'])
                        P.dma('sp', qgs[:, :], IN("qg")[:, :], writes=['qgs'])
                        P.dma('sp', kgs[:, :], IN("kg")[:, :], writes=['kgs'])
                        P.op('dve', lambda e: e.tensor_scalar(out=qgs[:, :], in0=qgs[:, :], scalar1=float(128 ** -0.5),
                                                              scalar2=None, op0=ALU.mult), reads=['qgs'], writes=['qgs'])
                        P.op('dve', lambda e: e.memset(ssq_a[:, :], 0.0), writes=['ssq_a'])
                        SL = slopes()

                        def qknormA(bank):
                            i = cnt['k'] % 2
                            cnt['k'] += 1
                            bkey = 'pb%d' % bank
                            P.op('act', lambda e: e.copy(out=kfb[i][:, :], in_=ps[bank][:, 0:256]),
                                 reads=[bkey], writes=['kf%d' % i])
                            P.op('pool', lambda e: e.tensor_tensor(out=sqb[i][:, :], in0=kfb[i][:, :], in1=kfb[i][:, :],
                                                                   op=ALU.mult), reads=['kf%d' % i], writes=['sqb%d' % i])
                            return i

                        def qknormB(i, dst_ap, gsc, gkey, dkey):
                            P.op('pe', lambda e: e.matmul(ps[2][:, 0:256], lhsT=ones_b[:, :], rhs=sqb[i][:, :],
                                                          start=True, stop=True),
                                 reads=['sqb%d' % i, 'ones_b'], writes=['pb2'])
                            P.op('dve', lambda e: e.tensor_scalar(out=v1[:, :], in0=ps[2][:, 0:256], scalar1=1.0 / 128,
                                                                  scalar2=EPS, op0=ALU.mult, op1=ALU.add),
                                 reads=['pb2'], writes=['v1'])
                            P.op('act', lambda e: e.activation(out=v2[:, :], in_=v1[:, :], func=AF.Sqrt),
                                 reads=['v1'], writes=['v2'])
                            P.op('dve', lambda e: e.reciprocal(out=v3[:, :], in_=v2[:, :]), reads=['v2'], writes=['v3'])
                            P.op('dve', lambda e: e.scalar_tensor_tensor(out=dst_ap, in0=kfb[i][:, :], scalar=gsc[:, 0:1],
                                                                         in1=v3[:, :], op0=ALU.mult, op1=ALU.mult),
                                 reads=['kf%d' % i, 'v3', gkey], writes=[dkey])

                        def load_pair_weights(hp):
                            h0 = 2 * hp
                            load_block(24 + h0, Wq[:, 0, :, :], 'Wq0')
                            load_block(24 + h0 + 1, Wq[:, 1, :, :], 'Wq1')
                            load_block(48 + h0, Wk[:, 0, :, :], 'Wk0')
                            load_block(48 + h0 + 1, Wk[:, 1, :, :], 'Wk1')
                            load_block(72 + h0, Wv[:, :, 0, :], 'Wv0')
                            load_block(72 + h0 + 1, Wv[:, :, 1, :], 'Wv1')

                        NHP = 12 if not LIM else 1
                        load_pair_weights(0)
                        for hp in range(NHP):
                            h0 = 2 * hp
                            for ch in range(12):
                                hb = load_hT(2 * ch)
                                ks = []
                                for hh in range(2):
                                    proj_fm(Wk[:, hh, :, :], 'Wk%d' % hh, hb, hh, 'pb%d' % hh)
                                    ks.append(qknormA(hh))
                                for t in range(2):
                                    def mmv(e, t=t, hb=hb):
                                        for kc in range(KC):
                                            r = e.matmul(ps[3][:, 0:256], lhsT=hTc[hb][:, t, kc, :], rhs=Wv[:, kc, :, :],
                                                         start=(kc == 0), stop=(kc == KC - 1))
                                        return r
                                    P.op('pe', mmv, reads=['Wv0', 'Wv1', 'hTc%d' % hb], writes=['pb3'])
                                    P.op('dve', lambda e, t=t, ch=ch: e.tensor_copy(out=Vsb[:, 2 * ch + t, :],
                                                                                    in_=ps[3][:, 0:256]),
                                         reads=['pb3'], writes=['V%d' % (2 * ch + t)])
                                for hh in range(2):
                                    qknormB(ks[hh], KT[:, hh, ch * 256:(ch + 1) * 256], kgs, 'kgs', 'KT%d_%d' % (hh, ch))
                                if ch >= 8:
                                    qs = []
                                    for hh in range(2):
                                        proj_fm(Wq[:, hh, :, :], 'Wq%d' % hh, hb, hh, 'pb%d' % hh)
                                        qs.append(qknormA(hh))
                                    for hh in range(2):
                                        qknormB(qs[hh], QT[:, hh, (ch - 8) * 256:(ch - 7) * 256], qgs, 'qgs',
                                                'QT%d_%d' % (hh, ch - 8))
                            if hp + 1 < NHP:
                                load_pair_weights(hp + 1)
                            for hh in range(2):
                                h = h0 + hh
                                for g in range(2):
                                    qb0 = 16 + 4 * g
                                    kaps = list(range(4 * g, 4 * g + 20))

                                    SB = (4, 5, 0, 1)
                                    SK = ('pS0', 'pS1', 'pb0', 'pb1')

                                    def issue_S(kap, sbk, hh=hh, g=g):
                                        P.op('pe', lambda e: e.matmul(ps[SB[sbk]][:, :], lhsT=KT[:, hh, kap * 128:(kap + 1) * 128],
                                                                      rhs=QT[:, hh, g * 512:(g + 1) * 512], start=True, stop=True),
                                             reads=['KT%d_%d' % (hh, kap // 2), 'QT%d_%d' % (hh, 2 * g), 'QT%d_%d' % (hh, 2 * g + 1)],
                                             writes=[SK[sbk]])
                                    sb0 = cnt['s']
                                    LA = 3
                                    for a in range(LA):
                                        issue_S(kaps[a], (sb0 + a) % 4)
                                    for idx, kap in enumerate(kaps):
                                        sbk = (sb0 + idx) % 4
                                        if idx + LA < len(kaps):
                                            issue_S(kaps[idx + LA], (sb0 + idx + LA) % 4)
                                        off = 128 * (qb0 - kap) + SOFF
                                        P.op('dve', lambda e, off=off, sbk=sbk, h=h: e.scalar_tensor_tensor(
                                            out=ssb[sbk][:, :], in0=tdel[:, off:off + 512], scalar=SL[h],
                                            in1=ps[SB[sbk]][:, :], op0=ALU.mult, op1=ALU.add),
                                            reads=[SK[sbk], 'tdel'], writes=['ssb%d' % sbk])
                                        P.op('act', lambda e, sbk=sbk, kap=kap: e.activation(
                                            out=eb[sbk][:, :], in_=ssb[sbk][:, :], func=AF.Exp, bias=kbias[:, kap:kap + 1]),
                                            reads=['ssb%d' % sbk, 'kbias'], writes=['eb%d' % sbk])
                                        P.op('pool', lambda e, sbk=sbk, off=off: e.tensor_tensor(
                                            out=pTb[sbk][:, :], in0=eb[sbk][:, :], in1=tmul[:, off:off + 512], op=ALU.mult),
                                            reads=['eb%d' % sbk, 'tmul'], writes=['pT%d' % sbk])

                                        def pv(e, sbk=sbk, kap=kap, idx=idx, hh=hh):
                                            e.matmul(ps[6][:, :], lhsT=Vsb[:, kap, hh * 128:(hh + 1) * 128], rhs=pTb[sbk][:, :],
                                                     start=(idx == 0), stop=(idx == 19))
                                            return e.matmul(ps[7][:, :], lhsT=ones_b[:, :], rhs=pTb[sbk][:, :],
                                                            start=(idx == 0), stop=(idx == 19))
                                        P.op('pe', pv, reads=['pT%d' % sbk, 'V%d' % kap, 'ones_b'], writes=['pO'])
                                    cnt['s'] = sb0 + len(kaps)
                                    P.op('dve', lambda e: e.reciprocal(out=rec[:, :], in_=ps[7][:, :]),
                                         reads=['pO'], writes=['rec'])
                                    P.op('dve', lambda e: e.tensor_tensor(out=yaf[:, :], in0=ps[6][:, :], in1=rec[:, :],
                                                                          op=ALU.mult), reads=['pO', 'rec'], writes=['yaf'])
                                    P.op('pool', lambda e: e.tensor_tensor(out=ysq[:, :], in0=yaf[:, :], in1=yaf[:, :],
                                                                           op=ALU.mult), reads=['yaf'], writes=['ysq'])
                                    P.op('pe', lambda e: e.matmul(ps[2][:, :], lhsT=ones_b[:, :], rhs=ysq[:, :],
                                                                  start=True, stop=True),
                                         reads=['ysq', 'ones_b'], writes=['pb2'])
                                    P.op('dve', lambda e, g=g: e.tensor_tensor(out=ssq_a[:, g * 512:(g + 1) * 512],
                                                                               in0=ps[2][:, :], in1=ssq_a[:, g * 512:(g + 1) * 512],
                                                                               op=ALU.add),
                                         reads=['pb2', 'ssq_a'], writes=['ssq_a'])
                                    yi = cnt['y'] % 2
                                    cnt['y'] += 1
                                    P.op('pool', lambda e, yi=yi, h=h: e.tensor_scalar(out=yst[yi][:, :], in0=yaf[:, :],
                                                                                       scalar1=gbr[:, 8 + h:9 + h], scalar2=None,
                                                                                       op0=ALU.mult),
                                         reads=['yaf', 'gbr'], writes=['yst%d' % yi])
                                    P.dma('sp', yT_d[:, 8 + h, g * 512:(g + 1) * 512], yst[yi][:, :],
                                          reads=['yst%d' % yi], writes=['yT_d%d_%d' % (8 + h, g)])
                        finish_rstd(ssq_a, 3072.0, rstd_a, 'ssq_a')
                        P.barrier()
                if dbg:
                    P.dma('sp', dbg_t[:, 0:8], rstd_c[:, :], reads=['ssq_crstd'], writes=['dbg'])
                    if stages >= 4:
                        P.dma('sp', dbg_t[:, 8:16], rstd_a[:, :], reads=['ssq_arstd'], writes=['dbg'])
                P.barrier()

        if stages >= 5:
            with ExitStack() as st:
                yT = sb("yT", [128, KC, TOK], BF16, stack=st)
                gt1b = sb("gt1b", [128, D], stack=st)
                wostg = [sb("wostg%d" % i, [128, 8, 512], stack=st) for i in range(2)]
                Wo2 = [sb("Wo%d" % i, [128, KC, 512], BF16, stack=st) for i in range(2)]
                xs = [sb("xs%d" % i, [128, 512], stack=st) for i in range(2)]
                t1 = [sb("t1_%d" % i, [128, 512], stack=st) for i in range(2)]
                x2s = [sb("x2s%d" % i, [128, 512], stack=st) for i in range(2)]
                for k8 in range(4):
                    P.dma('sp', yT[:, k8 * 8:(k8 + 1) * 8, :], yT_d[:, k8 * 8:(k8 + 1) * 8, :], writes=['yT%d' % k8])
                P.dma('sp', gt1b[:, :], gtb_d[0, :, :], writes=['gt1b'])
                nw = 0
                nt = 0
                def load_wo(cb):
                    nonlocal_nw = cnt.setdefault('wo', 0)
                    for k8 in range(4):
                        i = cnt['wo'] % 2
                        cnt['wo'] += 1
                        P.dma('sp', wostg[i][:, :, :],
                              IN("w_out")[k8 * 1024:(k8 + 1) * 1024, cb * 512:(cb + 1) * 512].rearrange('(k p) c -> p k c', p=128),
                              writes=['wostg%d' % i])
                        dst = Wo2[cb % 2][:, k8 * 8:(k8 + 1) * 8, :]
                        if i == 0:
                            P.op('pool', lambda e, dst=dst, i=i: e.tensor_copy(out=dst, in_=wostg[i][:, :, :]),
                                 reads=['wostg%d' % i], writes=['Wo%d_%d' % (cb % 2, k8)])
                        else:
                            P.op('act', lambda e, dst=dst, i=i: e.copy(out=dst, in_=wostg[i][:, :, :]),
                                 reads=['wostg%d' % i], writes=['Wo%d_%d' % (cb % 2, k8)])
                load_wo(0)
                for cb in range(8):
                    if cb + 1 < 8:
                        load_wo(cb + 1)
                    Wo = Wo2[cb % 2]
                    wkeys = ['Wo%d_%d' % (cb % 2, k8) for k8 in range(4)]
                    for tile in range(8):
                        j = nt % 2
                        nt += 1
                        bc, ba = (0, 1) if j == 0 else (2, 3)

                        def mmo(e, tile=tile, bc=bc, ba=ba, Wo=Wo):
                            for kc in range(8):
                                e.matmul(ps[bc][:, :], lhsT=yT[:, kc, tile * 128:(tile + 1) * 128], rhs=Wo[:, kc, :],
                                         start=(kc == 0), stop=(kc == 7))
                            for kc in range(8, KC):
                                r = e.matmul(ps[ba][:, :], lhsT=yT[:, kc, tile * 128:(tile + 1) * 128], rhs=Wo[:, kc, :],
                                             start=(kc == 8), stop=(kc == KC - 1))
                            return r
                        P.op('pe', mmo, reads=['yT0', 'yT1', 'yT2', 'yT3'] + wkeys, writes=['po%d' % j])
                        P.dma('sp', xs[j][:, :], IN("xw")[HALO + tile * 128:HALO + (tile + 1) * 128, cb * 512:(cb + 1) * 512],
                              writes=['xs%d' % j])
                        P.op('dve', lambda e, j=j, tile=tile, bc=bc: e.tensor_scalar(
                            out=t1[j][:, :], in0=ps[bc][:, :], scalar1=rstd_c[:, tile:tile + 1], scalar2=None, op0=ALU.mult),
                            reads=['po%d' % j], writes=['t1_%d' % j])
                        P.op('dve', lambda e, j=j, tile=tile, ba=ba: e.scalar_tensor_tensor(
                            out=t1[j][:, :], in0=ps[ba][:, :], scalar=rstd_a[:, tile:tile + 1], in1=t1[j][:, :],
                            op0=ALU.mult, op1=ALU.add), reads=['po%d' % j, 't1_%d' % j], writes=['t1_%d' % j])
                        P.op('dve', lambda e, j=j, cb=cb: e.tensor_tensor(
                            out=t1[j][:, :], in0=t1[j][:, :], in1=gt1b[:, cb * 512:(cb + 1) * 512], op=ALU.mult),
                            reads=['t1_%d' % j, 'gt1b'], writes=['t1_%d' % j])
                        P.op('dve', lambda e, j=j: e.tensor_tensor(out=x2s[j][:, :], in0=t1[j][:, :], in1=xs[j][:, :], op=ALU.add),
                             reads=['t1_%d' % j, 'xs%d' % j], writes=['x2s%d' % j])
                        P.dma('sp', out[tile * 128:(tile + 1) * 128, cb * 512:(cb + 1) * 512], x2s[j][:, :],
                              reads=['x2s%d' % j], writes=['out_%d_%d' % (tile, cb)])
                P.barrier()
            P.barrier()

        if stages >= 6:
            with ExitStack() as st:
                h2T = sb("h2T", [128, KC, TOK], BF16, stack=st)
                with ExitStack() as s5:
                    xt5 = sb("xt5", [128, D], stack=s5)
                    junk5 = sb("junk5", [128, D], BF16, stack=s5)
                    ss5 = sb("ss5", [128, 1], stack=s5)
                    rs5 = sb("rs5", [128, 2], stack=s5)
                    hTf = sb("hTf", [128, KC, 128], stack=s5)
                    wr = sb("wr", [128, KC, 72], stack=s5)
                    L = sb("L", [128, 72], stack=s5)
                    sm = sb("sm", [128, 16], stack=s5)
                    goh = sb("goh", [128, 8], stack=s5)
                    gex = sb("gex", [128, 8], stack=s5)
                    ein = sb("ein", [128, 8], stack=s5)
                    e2 = sb("e2", [128, 8], stack=s5)
                    oh1 = sb("oh1", [128, 8], stack=s5)
                    oh2 = sb("oh2", [128, 8], stack=s5)
                    wsel = sb("wsel", [128, 8], stack=s5)
                    wgt = sb("wgt", [128, 64], stack=s5)
                    wgtTs = sb("wgtTs", [64, 128], stack=s5)
                    P.dma('sp', wr[:, :, :].rearrange('p k c -> p (k c)'), IN("w_r")[:, :], writes=['wr'])
                    for tile in range(8):
                        key = 's5_'
                        norm_transpose(out[tile * 128:(tile + 1) * 128, :], xt5, junk5, ss5, rs5,
                                       (lambda kc, tile=tile: h2T[:, kc, tile * 128:(tile + 1) * 128]),
                                       A2, B2, key, (0, 1), hTf=hTf)

                        def mml(e):
                            for kc in range(KC):
                                r = e.matmul(ps[2][:, 0:72], lhsT=hTf[:, kc, :], rhs=wr[:, kc, :],
                                             start=(kc == 0), stop=(kc == KC - 1))
                            return r
                        P.op('pe', mml, reads=['wr'] + [key + 'hT%df' % kc for kc in range(KC)], writes=['pb2'])
                        V = lambda e: e
                        P.op('dve', lambda e: e.tensor_copy(out=L[:, :], in_=ps[2][:, 0:72]), reads=['pb2'], writes=['R'])
                        P.op('dve', lambda e: e.tensor_reduce(out=sm[:, 0:1], in_=L[:, 0:8], axis=AX.X, op=ALU.max),
                             reads=['R'], writes=['R'])
                        P.op('dve', lambda e: e.tensor_scalar(out=goh[:, :], in0=L[:, 0:8], scalar1=sm[:, 0:1], scalar2=None,
                                                              op0=ALU.is_equal), reads=['R'], writes=['R'])
                        P.op('dve', lambda e: e.tensor_scalar(out=sm[:, 1:2], in0=sm[:, 0:1], scalar1=-1.0, scalar2=None,
                                                              op0=ALU.mult), reads=['R'], writes=['R'])
                        P.op('dve', lambda e: e.memset(sm[:, 2:3], 0.0), reads=['R'], writes=['R'])
                        P.op('act', lambda e: e.activation(out=gex[:, :], in_=L[:, 0:8], func=AF.Exp, bias=sm[:, 1:2],
                                                           accum_out=sm[:, 2:3]), reads=['R'], writes=['R'])
                        P.op('dve', lambda e: e.reciprocal(out=sm[:, 3:4], in_=sm[:, 2:3]), reads=['R'], writes=['R'])
                        P.op('dve', lambda e: e.tensor_scalar(out=ein[:, :], in0=L[:, 8:16], scalar1=goh[:, 0:1], scalar2=None,
                                                              op0=ALU.mult), reads=['R'], writes=['R'])
                        for g in range(1, 8):
                            P.op('dve', lambda e, g=g: e.scalar_tensor_tensor(
                                out=ein[:, :], in0=L[:, 8 + 8 * g:16 + 8 * g], scalar=goh[:, g:g + 1], in1=ein[:, :],
                                op0=ALU.mult, op1=ALU.add), reads=['R'], writes=['R'])
                        P.op('dve', lambda e: e.tensor_reduce(out=sm[:, 4:5], in_=ein[:, :], axis=AX.X, op=ALU.max),
                             reads=['R'], writes=['R'])
                        P.op('dve', lambda e: e.tensor_scalar(out=oh1[:, :], in0=ein[:, :], scalar1=sm[:, 4:5], scalar2=None,
                                                              op0=ALU.is_equal), reads=['R'], writes=['R'])
                        P.op('dve', lambda e: e.scalar_tensor_tensor(out=e2[:, :], in0=oh1[:, :], scalar=-1.0e30, in1=ein[:, :],
                                                                     op0=ALU.mult, op1=ALU.add), reads=['R'], writes=['R'])
                        P.op('dve', lambda e: e.tensor_reduce(out=sm[:, 5:6], in_=e2[:, :], axis=AX.X, op=ALU.max),
                             reads=['R'], writes=['R'])
                        P.op('dve', lambda e: e.tensor_scalar(out=oh2[:, :], in0=e2[:, :], scalar1=sm[:, 5:6], scalar2=None,
                                                              op0=ALU.is_equal), reads=['R'], writes=['R'])
                        P.op('dve', lambda e: e.tensor_tensor(out=sm[:, 6:7], in0=sm[:, 5:6], in1=sm[:, 4:5], op=ALU.subtract),
                             reads=['R'], writes=['R'])
                        P.op('act', lambda e: e.activation(out=sm[:, 7:8], in_=sm[:, 6:7], func=AF.Exp), reads=['R'], writes=['R'])
                        P.op('dve', lambda e: e.tensor_scalar(out=sm[:, 8:9], in0=sm[:, 7:8], scalar1=1.0, scalar2=None,
                                                              op0=ALU.add), reads=['R'], writes=['R'])
                        P.op('dve', lambda e: e.reciprocal(out=sm[:, 9:10], in_=sm[:, 8:9]), reads=['R'], writes=['R'])
                        P.op('dve', lambda e: e.tensor_tensor(out=sm[:, 10:11], in0=sm[:, 7:8], in1=sm[:, 9:10], op=ALU.mult),
                             reads=['R'], writes=['R'])
                        P.op('dve', lambda e: e.tensor_scalar(out=sm[:, 9:11], in0=sm[:, 9:11], scalar1=sm[:, 3:4], scalar2=None,
                                                              op0=ALU.mult), reads=['R'], writes=['R'])
                        P.op('dve', lambda e: e.tensor_scalar(out=wsel[:, :], in0=oh1[:, :], scalar1=sm[:, 9:10], scalar2=None,
                                                              op0=ALU.mult), reads=['R'], writes=['R'])
                        P.op('dve', lambda e: e.scalar_tensor_tensor(out=wsel[:, :], in0=oh2[:, :], scalar=sm[:, 10:11],
                                                                     in1=wsel[:, :], op0=ALU.mult, op1=ALU.add),
                             reads=['R'], writes=['R'])
                        for g in range(8):
                            P.op('dve', lambda e, g=g: e.tensor_scalar(out=wgt[:, 8 * g:8 * g + 8], in0=wsel[:, :],
                                                                       scalar1=goh[:, g:g + 1], scalar2=None, op0=ALU.mult),
                                 reads=['R', 'wgtrd'], writes=['R'])
                        P.op('pe', lambda e: e.transpose(out=ps[3][0:64, 0:128], in_=wgt[:, 0:64], identity=ident[:, :]),
                             reads=['R', 'ident'], writes=['pb3', 'wgtrd'])
                        P.op('dve', lambda e: e.tensor_copy(out=wgtTs[:, :], in_=ps[3][0:64, 0:128]),
                             reads=['pb3'], writes=['wgtTs'])
                        P.dma('sp', wgtT_d[:, tile * 128:(tile + 1) * 128], wgtTs[:, :], reads=['wgtTs'], writes=['wgtT_d'])
                    P.barrier()
                if stages >= 7:
                    with ExitStack() as s6:
                        wstg6 = [sb("wstg6_%d" % i, [128, (KC // 2) * 128], stack=s6) for i in range(4)]
                        W13 = [[sb("W13_%d_%d" % (a, i), [128, KC, 128], BF16, stack=s6) for i in range(2)] for a in range(2)]
                        actT = sb("actT", [128, 2, 4, TOK], BF16, stack=s6)
                        wb = [sb("wb%d" % i, [128, TOK], stack=s6) for i in range(2)]
                        s1 = [sb("s1_%d" % i, [128, 512], stack=s6) for i in range(2)]
                        w2stg = [sb("w2stg%d" % i, [128, 4 * 512], stack=s6) for i in range(2)]
                        W2b = [sb("W2b%d" % i, [128, 2, 4, 512], BF16, stack=s6) for i in range(2)]
                        gt2s = [sb("gt2s%d" % i, [128, 512], stack=s6) for i in range(2)]
                        NOST = 6
                        ost = [sb("ost%d" % i, [128, 512], stack=s6) for i in range(NOST)]
                        NP = NEXP // 2 if not LIM else 1
                        steps = []
                        na = 0
                        nb = 0
                        for pair in range(NP):
                            for el in range(2):
                                for j in range(4):
                                    steps.append(('A', pair, el, j, na % 2))
                                    na += 1
                            for cb in range(8):
                                steps.append(('B', pair, cb, nb % 2))
                                nb += 1
                        cn = {'l': 0, 'w2': 0, 'o': 0}

                        def cast3(i, dst, src3, skey, dkey, engs):
                            eng = engs[i]
                            if eng == 'act':
                                P.op('act', lambda e: e.copy(out=dst, in_=src3), reads=[skey], writes=[dkey])
                            else:
                                P.op(eng, lambda e: e.tensor_copy(out=dst, in_=src3), reads=[skey], writes=[dkey])

                        def prefetch(stp):
                            if stp[0] == 'A':
                                _, pair, el, j, a = stp
                                ex = pair * 2 + el
                                if j == 0:
                                    P.dma('sp', wb[el][:, :], wgtT_d[ex:ex + 1, :].to_broadcast([128, TOK]), writes=['wb%d' % el])
                                for wi, wname in enumerate(("w1_t", "w3_t")):
                                    for hf in range(2):
                                        i = cn['l'] % 4
                                        cn['l'] += 1
                                        P.dma('sp', wstg6[i][:, :], IN(wname)[ex, j, :, hf * 2048:(hf + 1) * 2048], writes=['wstg6_%d' % i])
                                        src3 = wstg6[i][:, :].rearrange('p (k c) -> p k c', k=KC // 2)
                                        cast3((wi + hf) % 2, W13[a][wi][:, hf * 16:(hf + 1) * 16, :], src3, 'wstg6_%d' % i,
                                              'W13_%d_%d_%d' % (a, wi, hf), ('dve', 'act'))
                            else:
                                _, pair, cb, wa_ = stp
                                for el in range(2):
                                    ex = pair * 2 + el
                                    i = cn['w2'] % 2
                                    cn['w2'] += 1
                                    P.dma('sp', w2stg[i][:, :], IN("w2_t")[ex, cb, :, :], writes=['w2stg%d' % i])
                                    src3 = w2stg[i][:, :].rearrange('p (k c) -> p k c', k=4)
                                    cast3(i, W2b[wa_][:, el, :, :], src3, 'w2stg%d' % i, 'W2b%d_%d' % (wa_, el), ('pool', 'act'))
                                P.dma('sp', gt2s[wa_][:, :], gtb_d[1, :, cb * 512:(cb + 1) * 512], writes=['gt2s%d' % wa_])

                        def compute(stp):
                            if stp[0] == 'A':
                                _, pair, el, j, a = stp

                                for chk in range(2):
                                    def mm13(e, chk=chk):
                                        for wi in range(2):
                                            for kc in range(KC):
                                                r = e.matmul(ps[2 * chk + wi][:, :], lhsT=W13[a][wi][:, kc, :],
                                                             rhs=h2T[:, kc, chk * 512:(chk + 1) * 512],
                                                             start=(kc == 0), stop=(kc == KC - 1))
                                        return r
                                    P.op('pe', mm13, reads=['W13_%d_%d_%d' % (a, wi_, hf_) for wi_ in range(2) for hf_ in range(2)] + ['h2T'], writes=['pa%d' % chk])
                                for chk in range(2):
                                    P.op('act', lambda e, chk=chk: e.activation(out=s1[chk][:, :], in_=ps[2 * chk][:, :], func=AF.Silu),
                                         reads=['pa%d' % chk], writes=['s1_%d' % chk])
                                    P.op('dve', lambda e, chk=chk: e.tensor_tensor(out=s1[chk][:, :], in0=ps[2 * chk + 1][:, :],
                                                                                  in1=s1[chk][:, :], op=ALU.mult),
                                         reads=['pa%d' % chk, 's1_%d' % chk], writes=['s1_%d' % chk])
                                    P.op('pool', lambda e, chk=chk: e.tensor_tensor(
                                        out=actT[:, el, j, chk * 512:(chk + 1) * 512], in0=s1[chk][:, :],
                                        in1=wb[el][:, chk * 512:(chk + 1) * 512], op=ALU.mult),
                                        reads=['s1_%d' % chk, 'wb%d' % el], writes=['actT'])
                            else:
                                _, pair, cb, wa_ = stp
                                for tile in range(8):
                                    o = cn['o'] % 2
                                    ob = cn['o'] % NOST
                                    cn['o'] += 1

                                    def mm2(e, tile=tile, o=o):
                                        n = 0
                                        for el in range(2):
                                            for kc in range(4):
                                                r = e.matmul(ps[4 + o][:, :], lhsT=actT[:, el, kc, tile * 128:(tile + 1) * 128],
                                                             rhs=W2b[wa_][:, el, kc, :], start=(n == 0), stop=(n == 7))
                                                n += 1
                                        return r
                                    P.op('pe', mm2, reads=['actT', 'W2b%d_0' % wa_, 'W2b%d_1' % wa_], writes=['pm%d' % o])
                                    P.op('dve', lambda e, o=o, ob=ob: e.tensor_tensor(out=ost[ob][:, :], in0=ps[4 + o][:, :],
                                                                                     in1=gt2s[wa_][:, :], op=ALU.mult),
                                         reads=['pm%d' % o, 'gt2s%d' % wa_], writes=['ost%d' % ob])
                                    P.dma('pool', out[tile * 128:(tile + 1) * 128, cb * 512:(cb + 1) * 512], ost[ob][:, :],
                                          reads=['ost%d' % ob], writes=['out_%d_%d' % (tile, cb)], accum_op=ALU.add)

                        prefetch(steps[0])
                        for si, stp in enumerate(steps):
                            if si + 1 < len(steps):
                                prefetch(steps[si + 1])
                            compute(stp)
                        P.barrier()
            P.barrier()

        P.barrier()
    return nc, sorted(_in)


def _mult(delta):
    d = np.asarray(delta)
    m = ((d >= 0) & (d <= 128)).astype(np.float32)
    m += ((d >= 0) & (d <= 512) & (d % 4 == 0)).astype(np.float32)
    m += ((d >= 0) & (d <= 2048) & (d % 16 == 0)).astype(np.float32)
    return m


def host_consts():
    import ml_dtypes
    u = np.arange(128)[:, None]
    cidx = np.arange(SLEN)[None, :]
    delta = cidx - SOFF - u
    tdelta = (-np.maximum(delta, 0)).astype(np.float32)
    tmult = _mult(delta).astype(ml_dtypes.bfloat16)
    return {"ident": np.eye(128, dtype=np.float32), "tdelta": np.ascontiguousarray(tdelta),
            "tmult": np.ascontiguousarray(tmult)}


def t32(v):
    return np.ascontiguousarray(np.asarray(v, np.float32).reshape(KC, 128).T)


def host_shared(inp, need):
    sh = {}
    if "c_t" in need:
        sh["c_t"] = t32(inp["c"][0])
    if "w_ada" in need:
        sh["w_ada"] = np.ascontiguousarray(inp["w_ada"][0])
    if "b_ada" in need:
        sh["b_ada"] = np.ascontiguousarray(inp["b_ada"][0][None, :])
    if "g_mix_t" in need:
        sh["g_mix_t"] = t32(inp["g_mix"][0])
    if "g_ffn_t" in need:
        sh["g_ffn_t"] = t32(inp["g_ffn"][0])
    if "g_br_t" in need:
        sh["g_br_t"] = t32(inp["g_branch"][0])
    if "w_in_t" in need:
        w = inp["w_in"][0].reshape(KC, 128, 96, 128).transpose(2, 1, 0, 3)
        sh["w_in_t"] = np.ascontiguousarray(w).reshape(96, 128, KC * 128)
    if "conv_w_t" in need:
        cw = inp["conv_w"][0].reshape(3, 8, 128).transpose(2, 1, 0)
        sh["conv_w_t"] = np.ascontiguousarray(cw).reshape(128, 24)
    if "qg" in need:
        sh["qg"] = np.ascontiguousarray(inp["q_norm_g"][0][:, None])
    if "kg" in need:
        sh["kg"] = np.ascontiguousarray(inp["k_norm_g"][0][:, None])
    if "w_out" in need:
        sh["w_out"] = np.ascontiguousarray(inp["w_out"][0])
    if "w_r" in need:
        wr = np.concatenate([inp["w_group"][0], inp["w_expert"][0]], axis=1)
        sh["w_r"] = np.ascontiguousarray(wr.reshape(KC, 128, 72).transpose(1, 0, 2)).reshape(128, KC * 72)
    for nm, src in (("w1_t", "w1"), ("w3_t", "w3")):
        if nm in need:
            w = inp[src][0].reshape(NEXP, KC, 128, 4, 128).transpose(0, 3, 2, 1, 4)
            sh[nm] = np.ascontiguousarray(w).reshape(NEXP, 4, 128, KC * 128)
    if "w2_t" in need:
        w = inp["w2"][0].reshape(NEXP, 4, 128, 8, 512).transpose(0, 3, 2, 1, 4)
        sh["w2_t"] = np.ascontiguousarray(w).reshape(NEXP, 8, 128, 4 * 512)
    hc = host_consts()
    for k in hc:
        if k in need:
            sh[k] = hc[k]
    return sh


def host_core(inp, core, need):
    m = {}
    t0 = core * TOK - HALO
    if "xw" in need:
        x = inp["x"][0]
        xw = np.zeros((WIN, D), np.float32)
        lo = max(t0, 0)
        xw[lo - t0:] = x[lo:t0 + WIN]
        m["xw"] = xw
    if "keybias" in need:
        pos = t0 + np.arange(WIN)
        kb = np.where(pos >= 0, 0.0, NEG).astype(np.float32)
        m["keybias"] = np.ascontiguousarray(kb.reshape(NWT, 128).T)
    if "cmask" in need:
        m["cmask"] = np.full((128, 2), 1.0 if core > 0 else 0.0, np.float32)
    return m


_CACHE = {}


def kernel(**inputs):
    if "nc" not in _CACHE:
        _CACHE["nc"] = build()
    nc, need = _CACHE["nc"]
    sh = host_shared(inputs, need)
    in_maps = []
    for c in range(NCORES):
        m = dict(sh)
        m.update(host_core(inputs, c, need))
        in_maps.append(m)
    res = run_bass_kernel_spmd(nc, in_maps, core_ids=list(range(NCORES)))
    return np.concatenate([np.asarray(r["out"]) for r in res.results], axis=0).reshape(1, S, D).astype(np.float32)
```

```python
import numpy as np
from contextlib import ExitStack
import concourse.bass as bass
import concourse.mybir as mybir
from concourse.bass_utils import run_bass_kernel_spmd

F32 = mybir.dt.float32
BF16 = mybir.dt.bfloat16
ALU = mybir.AluOpType
AF = mybir.ActivationFunctionType
AX = mybir.AxisListType

NCORES = 8
D = 4096
S = 8192
TOK = S // NCORES
HALO = 2048
WIN = TOK + HALO
NWT = WIN // 128
KC = D // 128
NH = 24
EPS = 1e-6
NEG = -30000.0
SOFF = 384
SLEN = SOFF + 2048 + 512
NEXP = 64
import os
LIM = int(os.environ.get('KLIM', '0'))
SKIP = os.environ.get('KSKIP', '')
LIMM = int(os.environ.get('KLIMM', '0'))


class Prog:
    NQ = 10

    def __init__(self, nc, es):
        self.nc = nc
        self.engs = {'pe': nc.tensor, 'dve': nc.vector, 'act': nc.scalar,
                     'pool': nc.gpsimd, 'sp': nc.sync}
        self.sems = []
        self.csem = {}
        self.ccnt = {}
        for e in ['pe', 'dve', 'act', 'pool']:
            self.csem[e] = self._newsem(es, 'c_' + e)
            self.ccnt[e] = 0
        self.dsem = {}
        self.dcnt = {}
        self.dnext = {}
        for q in ['sp', 'act', 'pool']:
            self.dsem[q] = [self._newsem(es, 'd_%s%d' % (q, i)) for i in range(self.NQ)]
            self.dcnt[q] = [0] * self.NQ
            self.dnext[q] = 0
        self.seen = {e: {} for e in self.engs}
        self.lastw = {}
        self.readers = {}
        self.nins = 0
        self.pe_pending_wait = False
        self.pe_dummy = None

    def _newsem(self, es, name):
        h = es.enter_context(self.nc.semaphore(name))
        self.sems.append(h)
        return len(self.sems) - 1

    def _wait(self, e, tok):
        si, val = tok
        if e == 'pe' and si == self.csem['pe']:
            return
        if self.seen[e].get(si, 0) >= val:
            return
        if e == 'pe' and self.pe_pending_wait and self.pe_dummy is not None:
            self.engs[e].ldweights(self.pe_dummy)
        self.engs[e].wait_ge(self.sems[si], val)
        if e == 'pe':
            self.pe_pending_wait = True
        self.seen[e][si] = val

    def _deps(self, reads, writes):
        deps = {}
        def add(tok):
            if tok is None:
                return
            si, v = tok
            if deps.get(si, 0) < v:
                deps[si] = v
        for k in reads:
            add(self.lastw.get(k))
        for k in writes:
            add(self.lastw.get(k))
            for t in self.readers.get(k, ()):
                add(t)
        return list(deps.items())

    def _record(self, tok, reads, writes):
        for k in reads:
            self.readers.setdefault(k, []).append(tok)
        for k in writes:
            self.lastw[k] = tok
            self.readers[k] = []

    def op(self, e, fn, reads=(), writes=()):
        for tok in self._deps(reads, writes):
            self._wait(e, tok)
        ins = fn(self.engs[e])
        if e == 'pe':
            self.pe_pending_wait = False
        self.ccnt[e] += 1
        ins.then_inc(self.sems[self.csem[e]], 1)
        self._record((self.csem[e], self.ccnt[e]), reads, writes)
        self.nins += 1

    def dma(self, q, out, in_, reads=(), writes=(), **kw):
        i = self.dnext[q]
        self.dnext[q] = (i + 1) % self.NQ
        si = self.dsem[q][i]
        if self.dcnt[q][i]:
            self._wait(q, (si, self.dcnt[q][i]))
        for tok in self._deps(reads, writes):
            self._wait(q, tok)
        ins = self.engs[q].dma_start(out=out, in_=in_, **kw)
        self.dcnt[q][i] += 16
        ins.then_inc(self.sems[si], 16)
        self._record((si, self.dcnt[q][i]), reads, writes)
        self.nins += 1

    def barrier(self):
        toks = []
        for e in self.csem:
            if self.ccnt[e]:
                toks.append((self.csem[e], self.ccnt[e]))
        for q in self.dsem:
            for i in range(self.NQ):
                if self.dcnt[q][i]:
                    toks.append((self.dsem[q][i], self.dcnt[q][i]))
        for e in self.engs:
            for t in toks:
                if e in self.csem and t[0] == self.csem[e]:
                    continue
                self._wait(e, t)
        self.lastw = {}
        self.readers = {}


def slopes():
    return [float(2.0 ** (-8.0 * (h + 1) / NH)) for h in range(NH)]


def build(stages=99, dbg=None):
    nc = bass.Bass("TRN2", target_bir_lowering=False)

    def din(name, shape, dt=F32):
        return nc.dram_tensor(name, list(shape), dt, kind="ExternalInput").ap()

    dbg = set(dbg or ())

    INSHAPES = {
        "xw": ([WIN, D], F32), "c_t": ([128, KC], F32), "w_ada": ([D, 6 * D], F32),
        "b_ada": ([1, 6 * D], F32), "g_mix_t": ([128, KC], F32), "g_ffn_t": ([128, KC], F32),
        "g_br_t": ([128, KC], F32), "w_in_t": ([96, 128, KC * 128], F32),
        "conv_w_t": ([128, 24], F32), "qg": ([128, 1], F32), "kg": ([128, 1], F32),
        "w_out": ([D, D], F32), "w_r": ([128, KC * 72], F32),
        "w1_t": ([NEXP, 4, 128, KC * 128], F32), "w3_t": ([NEXP, 4, 128, KC * 128], F32),
        "w2_t": ([NEXP, 8, 128, 4 * 512], F32), "ident": ([128, 128], F32),
        "tdelta": ([128, SLEN], F32), "tmult": ([128, SLEN], BF16),
        "keybias": ([128, NWT], F32), "cmask": ([128, 2], F32),
    }
    _in = {}

    def IN(name):
        if name not in _in:
            shp, dt = INSHAPES[name]
            _in[name] = din(name, shp, dt)
        return _in[name]

    out = nc.dram_tensor("out", [TOK, D], F32, kind="ExternalOutput").ap()
    dbg_t = None
    if dbg:
        dbg_t = nc.dram_tensor("dbg", [128, 256], F32, kind="ExternalOutput").ap()

    def dscr(name, shape, dt):
        kind = "ExternalOutput" if name in dbg else "Internal"
        return nc.dram_tensor(name, list(shape), dt, kind=kind).ap()

    hT_d = dscr("hT_d", [NWT, 128, KC * 128], BF16)
    yT_d = dscr("yT_d", [128, KC, TOK], BF16)
    gtb_d = dscr("gtb_d", [2, 128, D], F32)
    wgtT_d = dscr("wgtT_d", [NEXP, TOK], F32)

    es = ExitStack()
    with es:
        P = Prog(nc, es)

        def sb(name, shape, dt=F32, stack=es):
            return stack.enter_context(nc.sbuf_tensor(name, list(shape), dt))

        ps = [es.enter_context(nc.psum_tensor("ps%d" % i, [128, 512], F32)) for i in range(8)]

        ident = sb("ident_sb", [128, 128])
        ones_f = sb("ones_f", [128, 128])
        ones_b = sb("ones_b", [128, 128], BF16)
        modT = sb("modT", [128, 192])
        A1 = sb("A1", [128, KC])
        A2 = sb("A2", [128, KC])
        gbr = sb("gbr", [128, KC])
        tmp32 = sb("tmp32", [128, KC])
        rstd_c = sb("rstd_c", [128, 8])
        rstd_a = sb("rstd_a", [128, 8])

        P.dma('sp', ident[:, :], IN("ident")[:, :], writes=['ident'])
        P.op('dve', lambda e: e.memset(ones_f[:, :], 1.0), writes=['ones_f'])
        P.op('dve', lambda e: e.memset(ones_b[:, :], 1.0), writes=['ones_b'])
        P.dma('sp', gbr[:, :], IN("g_br_t")[:, :], writes=['gbr'])

        def B1(kc):
            return modT[:, 0 * KC + kc:0 * KC + kc + 1]

        def B2(kc):
            return modT[:, 3 * KC + kc:3 * KC + kc + 1]

        with ExitStack() as st:
            c_sb = sb("c_sb", [128, KC], stack=st)
            sc = sb("sc", [128, KC], stack=st)
            wa = [sb("wa%d" % i, [128, 2048], stack=st) for i in range(3)]
            brow = sb("brow", [1, 2048], stack=st)
            row = sb("row", [1, 2048], stack=st)
            gstage = sb("gstage", [128, 2048], stack=st)
            gm = sb("gm", [128, KC], stack=st)
            gf = sb("gf", [128, KC], stack=st)

            P.dma('sp', c_sb[:, :], IN("c_t")[:, :], writes=['c_sb'])
            P.dma('sp', gm[:, :], IN("g_mix_t")[:, :], writes=['gm'])
            P.dma('sp', gf[:, :], IN("g_ffn_t")[:, :], writes=['gf'])
            P.op('act', lambda e: e.activation(out=sc[:, :], in_=c_sb[:, :], func=AF.Silu),
                 reads=['c_sb'], writes=['sc'])
            nd = 0
            for cb2 in (range(12) if not LIMM else (0, 4)):
                c0 = cb2 * 2048
                P.dma('sp', brow[0:1, :], IN("b_ada")[0:1, c0:c0 + 2048], writes=['brow'])
                for kc in range(KC):
                    buf = nd % 3
                    nd += 1
                    P.dma('sp', wa[buf][:, :], IN("w_ada")[kc * 128:(kc + 1) * 128, c0:c0 + 2048],
                          writes=['wa%d' % buf])

                    def mm(e, kc=kc, buf=buf):
                        for n in range(4):
                            r = e.matmul(ps[n][0:1, :], lhsT=sc[:, kc:kc + 1],
                                         rhs=wa[buf][:, n * 512:(n + 1) * 512],
                                         start=(kc == 0), stop=(kc == KC - 1))
                        return r
                    P.op('pe', mm, reads=['sc', 'wa%d' % buf], writes=['psM'])
                for n in range(4):
                    P.op('dve', lambda e, n=n: e.tensor_tensor(
                        out=row[0:1, n * 512:(n + 1) * 512], in0=ps[n][0:1, :],
                        in1=brow[0:1, n * 512:(n + 1) * 512], op=ALU.add),
                        reads=['psM', 'brow'], writes=['row'])

                def tr(e, cb2=cb2):
                    for q in range(16):
                        j = cb2 * 16 + q
                        r = e.matmul(ps[4][:, j:j + 1], lhsT=row[0:1, q * 128:(q + 1) * 128],
                                     rhs=ones_f[0:1, 0:1], start=True, stop=True)
                    return r
                P.op('pe', tr, reads=['row', 'ones_f'], writes=['psT'])
                if cb2 in (4, 5, 10, 11):
                    which = 0 if cb2 < 6 else 1
                    gc0 = (cb2 % 2) * 2048

                    def bc(e):
                        for n in range(4):
                            r = e.matmul(ps[n][:, :], lhsT=ones_f[0:1, :],
                                         rhs=row[0:1, n * 512:(n + 1) * 512], start=True, stop=True)
                        return r
                    P.op('pe', bc, reads=['row', 'ones_f'], writes=['psM'])
                    for n in range(4):
                        P.op('act', lambda e, n=n: e.copy(out=gstage[:, n * 512:(n + 1) * 512],
                                                          in_=ps[n][:, :]),
                             reads=['psM'], writes=['gstage'])
                    P.dma('sp', gtb_d[which, :, gc0:gc0 + 2048], gstage[:, :],
                          reads=['gstage'], writes=['gtb_d'])
            if LIMM:
                P.op('dve', lambda e: e.memset(ps[4][:, 0:192], 0.5), writes=['psT'])
            P.op('dve', lambda e: e.tensor_copy(out=modT[:, :], in_=ps[4][:, 0:192]),
                 reads=['psT'], writes=['modT'])
            P.op('dve', lambda e: e.tensor_scalar(out=tmp32[:, :], in0=modT[:, KC:2 * KC], scalar1=1.0,
                                                  scalar2=None, op0=ALU.add),
                 reads=['modT'], writes=['tmp32'])
            P.op('dve', lambda e: e.tensor_tensor(out=A1[:, :], in0=tmp32[:, :], in1=gm[:, :], op=ALU.mult),
                 reads=['tmp32', 'gm'], writes=['A1'])
            P.op('dve', lambda e: e.tensor_scalar(out=tmp32[:, :], in0=modT[:, 4 * KC:5 * KC], scalar1=1.0,
                                                  scalar2=None, op0=ALU.add),
                 reads=['modT', 'A1'], writes=['tmp32'])
            P.op('dve', lambda e: e.tensor_tensor(out=A2[:, :], in0=tmp32[:, :], in1=gf[:, :], op=ALU.mult),
                 reads=['tmp32', 'gf'], writes=['A2'])
            P.barrier()

        if dbg:
            P.dma('sp', dbg_t[:, 0:192], modT[:, :], reads=['modT'], writes=['dbg'])
            P.dma('sp', dbg_t[:, 192:224], A1[:, :], reads=['A1'], writes=['dbg'])
            P.dma('sp', dbg_t[:, 224:256], A2[:, :], reads=['A2'], writes=['dbg'])

        def norm_transpose(src_ap, xt, junk, ss, rs, hTdst, Aap, Bfn, key, pb, hTf=None):
            P.dma('sp', xt[:, :], src_ap, reads=[key + 'src'], writes=[key + 'xt'])
            P.op('dve', lambda e: e.memset(ss[:, :], 0.0), writes=[key + 'ss'])
            P.op('act', lambda e: e.activation(out=junk[:, :], in_=xt[:, :], func=AF.Square,
                                               accum_out=ss[:, 0:1]),
                 reads=[key + 'xt', key + 'ss'], writes=[key + 'junk', key + 'ss'])
            P.op('act', lambda e: e.activation(out=rs[:, 0:1], in_=ss[:, 0:1], func=AF.Sqrt,
                                               scale=1.0 / D, bias=EPS),
                 reads=[key + 'ss'], writes=[key + 'rs'])
            P.op('dve', lambda e: e.reciprocal(out=rs[:, 1:2], in_=rs[:, 0:1]),
                 reads=[key + 'rs'], writes=[key + 'rstd'])
            P.op('dve', lambda e: e.tensor_scalar(out=xt[:, :], in0=xt[:, :], scalar1=rs[:, 1:2],
                                                  scalar2=None, op0=ALU.mult),
                 reads=[key + 'xt', key + 'rstd'], writes=[key + 'xt'])
            for k4 in range(KC // 4 if 'tp' not in SKIP else 0):
                bank = pb[k4 % 2]
                bk = key + 'pst%d' % (k4 % 2)

                def tp(e, k4=k4, bank=bank):
                    for q in range(4):
                        kc = k4 * 4 + q
                        r = e.transpose(out=ps[bank][:, q * 128:(q + 1) * 128],
                                        in_=xt[:, kc * 128:(kc + 1) * 128], identity=ident[:, :])
                    return r
                P.op('pe', tp, reads=[key + 'xt', 'ident'], writes=[bk])
                for q in range(4 if 'evac' not in SKIP else 0):
                    kc = k4 * 4 + q
                    hk = key + 'hT%d' % kc
                    if hTf is not None:
                        P.op('act', lambda e, kc=kc, q=q, bank=bank: e.activation(
                            out=hTf[:, kc, :], in_=ps[bank][:, q * 128:(q + 1) * 128], func=AF.Identity,
                            scale=Aap[:, kc:kc + 1], bias=Bfn(kc)),
                            reads=[bk, 'A1', 'A2', 'modT'], writes=[hk + 'f'])
                        P.op('pool', lambda e, kc=kc: e.tensor_copy(out=hTdst(kc), in_=hTf[:, kc, :]),
                             reads=[hk + 'f'], writes=[hk])
                    elif k4 % 2 == 0:
                        P.op('dve', lambda e, kc=kc, q=q, bank=bank: e.tensor_scalar(
                            out=hTdst(kc), in0=ps[bank][:, q * 128:(q + 1) * 128],
                            scalar1=Aap[:, kc:kc + 1], scalar2=Bfn(kc), op0=ALU.mult, op1=ALU.add),
                            reads=[bk, 'A1', 'A2', 'modT'], writes=[hk])
                    else:
                        P.op('act', lambda e, kc=kc, q=q, bank=bank: e.activation(
                            out=hTdst(kc), in_=ps[bank][:, q * 128:(q + 1) * 128], func=AF.Identity,
                            scale=Aap[:, kc:kc + 1], bias=Bfn(kc)),
                            reads=[bk, 'A1', 'A2', 'modT'], writes=[hk])

        if stages >= 2:
            with ExitStack() as st:
                xts = [sb("xt%d" % i, [128, D], stack=st) for i in range(2)]
                junk = sb("junk", [128, D], BF16, stack=st)
                sss = [sb("ss%d" % i, [128, 1], stack=st) for i in range(2)]
                rss = [sb("rs%d" % i, [128, 2], stack=st) for i in range(2)]
                hTs = [sb("hTs%d" % i, [128, KC, 128], BF16, stack=st) for i in range(2)]
                for t in range(NWT):
                    b = t % 2
                    key = 's1_%d_' % b
                    norm_transpose(IN("xw")[t * 128:(t + 1) * 128, :], xts[b], junk, sss[b], rss[b],
                                   (lambda kc, b=b: hTs[b][:, kc, :]), A1, B1, key, (0, 1))
                    if 'store' in SKIP:
                        continue
                    P.dma('sp', hT_d[t, :, :], hTs[b][:, :, :].rearrange('p k t -> p (k t)'),
                          reads=[key + 'hT%d' % kc for kc in range(KC)], writes=['hT_d%d' % t])
                P.barrier()

        def W_IN(cb):
            return IN("w_in_t")[cb, :, :]

        cnt = {'l': 0, 'h': 0, 'b': 0, 'k': 0, 's': 0, 'y': 0}

        if stages >= 3:
            with ExitStack() as st:
                stg = [sb("wstg%d" % i, [128, KC * 128], stack=st) for i in range(2)]
                Wq = sb("Wq", [128, 2, KC, 128], BF16, stack=st)
                Wk = sb("Wk", [128, 2, KC, 128], BF16, stack=st)
                Wv = sb("Wv", [128, KC, 2, 128], BF16, stack=st)
                hTc = [sb("hTc%d" % i, [128, 2, KC, 128], BF16, stack=st) for i in range(2)]
                ssq_c = sb("ssq_c", [128, TOK], stack=st)
                ssq_a = sb("ssq_a", [128, TOK], stack=st)
                row1 = sb("row1", [1, TOK], stack=st)

                def load_block(cb, dst3, dkey):
                    i = cnt['l'] % 2
                    cnt['l'] += 1
                    P.dma('sp', stg[i][:, :], W_IN(cb), writes=['stg%d' % i])
                    src3 = stg[i][:, :].rearrange('p (k c) -> p k c', k=KC)
                    if i == 0:
                        P.op('pool', lambda e: e.tensor_copy(out=dst3, in_=src3), reads=['stg%d' % i], writes=[dkey])
                    else:
                        P.op('act', lambda e: e.copy(out=dst3, in_=src3), reads=['stg%d' % i], writes=[dkey])

                def load_hT(t0):
                    hb = cnt['h'] % 2
                    cnt['h'] += 1
                    P.dma('sp', hTc[hb][:, :, :, :].rearrange('p t k c -> p t (k c)'),
                          hT_d[t0:t0 + 2, :, :].rearrange('t p f -> p t f'), writes=['hTc%d' % hb])
                    return hb

                def proj_fm(wsrc, wkey, hb, bank, bkey):
                    def mm(e):
                        for kc in range(KC):
                            r = e.matmul(ps[bank][:, 0:256], lhsT=wsrc[:, kc, :], rhs=hTc[hb][:, :, kc, :],
                                         start=(kc == 0), stop=(kc == KC - 1))
                        return r
                    P.op('pe', mm, reads=[wkey, 'hTc%d' % hb], writes=[bkey])

                def finish_rstd(ssq, nfeat, dst, key):
                    P.op('dve', lambda e: e.tensor_scalar(out=row1[0:1, :], in0=ssq[0:1, :], scalar1=1.0 / nfeat,
                                                          scalar2=EPS, op0=ALU.mult, op1=ALU.add),
                         reads=[key], writes=['row1'])
                    P.op('act', lambda e: e.activation(out=row1[0:1, :], in_=row1[0:1, :], func=AF.Sqrt),
                         reads=['row1'], writes=['row1'])
                    P.op('dve', lambda e: e.reciprocal(out=row1[0:1, :], in_=row1[0:1, :]),
                         reads=['row1'], writes=['row1'])

                    def tr(e):
                        for t in range(8):
                            r = e.matmul(ps[2][:, t:t + 1], lhsT=row1[0:1, t * 128:(t + 1) * 128],
                                         rhs=ones_f[0:1, 0:1], start=True, stop=True)
                        return r
                    P.op('pe', tr, reads=['row1', 'ones_f'], writes=['pb2'])
                    P.op('dve', lambda e: e.tensor_copy(out=dst[:, :], in_=ps[2][:, 0:8]),
                         reads=['pb2'], writes=[key + 'rstd'])

                if LIM:
                    zt = sb('zt', [128, TOK], BF16, stack=st)
                    P.op('dve', lambda e: e.memset(zt[:, :], 0.0), writes=['zt'])
                    for k in range(KC):
                        P.dma('sp', yT_d[:, k, :], zt[:, :], reads=['zt'], writes=['yT_d%d' % k, 'yT_d%d_0' % k, 'yT_d%d_1' % k])
                with ExitStack() as s2:
                    u = sb("u", [128, 1280], stack=s2)
                    Bsb = sb("Bsb", [128, 1280], stack=s2)
                    Csb = sb("Csb", [128, 256], stack=s2)
                    tcv = sb("tcv", [128, TOK], stack=s2)
                    yc = sb("yc", [128, TOK], stack=s2)
                    ycsq = sb("ycsq", [128, TOK], BF16, stack=s2)
                    ycg = sb("ycg", [128, TOK], BF16, stack=s2)
                    cw = sb("cw", [128, 24], stack=s2)
                    cmask = sb("cmask_sb", [128, 2], stack=s2)
                    P.dma('sp', cw[:, :], IN("conv_w_t")[:, :], writes=['cw'])
                    P.dma('sp', cmask[:, :], IN("cmask")[:, :], writes=['cmask'])
                    P.op('dve', lambda e: e.memset(ssq_c[:, :], 0.0), writes=['ssq_c'])
                    for cblk in range(8 if not LIM else 1):
                        load_block(cblk, Wq[:, 0, :, :], 'Wq0')
                        load_block(8 + cblk, Wq[:, 1, :, :], 'Wq1')
                        load_block(16 + cblk, Wk[:, 0, :, :], 'Wk0')
                        for ch in range(5):
                            hb = load_hT(14 + 2 * ch)
                            c0 = ch * 256
                            proj_fm(Wq[:, 0, :, :], 'Wq0', hb, 0, 'pb0')
                            proj_fm(Wq[:, 1, :, :], 'Wq1', hb, 1, 'pb1')
                            proj_fm(Wk[:, 0, :, :], 'Wk0', hb, 3, 'pb3')
                            P.op('act', lambda e, c0=c0: e.copy(out=Bsb[:, c0:c0 + 256], in_=ps[0][:, 0:256]),
                                 reads=['pb0'], writes=['Bsb'])
                            P.op('act', lambda e: e.copy(out=Csb[:, :], in_=ps[1][:, 0:256]),
                                 reads=['pb1'], writes=['Csb'])
                            P.op('dve', lambda e, c0=c0: e.tensor_tensor(out=u[:, c0:c0 + 256], in0=ps[3][:, 0:256],
                                                                         in1=Csb[:, :], op=ALU.mult),
                                 reads=['pb3', 'Csb'], writes=['u'])
                        P.op('dve', lambda e: e.tensor_tensor(out=u[:, 254:256], in0=u[:, 254:256], in1=cmask[:, :],
                                                              op=ALU.mult), reads=['u', 'cmask'], writes=['u'])

                        def cwc(j, cblk=cblk):
                            return cw[:, cblk * 3 + j:cblk * 3 + j + 1]
                        P.op('dve', lambda e: e.tensor_scalar(out=tcv[:, :], in0=u[:, 256:1280], scalar1=cwc(2),
                                                              scalar2=None, op0=ALU.mult),
                             reads=['u', 'cw'], writes=['tcv'])
                        P.op('dve', lambda e: e.scalar_tensor_tensor(out=tcv[:, :], in0=u[:, 255:1279], scalar=cwc(1),
                                                                     in1=tcv[:, :], op0=ALU.mult, op1=ALU.add),
                             reads=['u', 'cw', 'tcv'], writes=['tcv'])
                        P.op('dve', lambda e: e.scalar_tensor_tensor(out=tcv[:, :], in0=u[:, 254:1278], scalar=cwc(0),
                                                                     in1=tcv[:, :], op0=ALU.mult, op1=ALU.add),
                             reads=['u', 'cw', 'tcv'], writes=['tcv'])
                        P.op('dve', lambda e: e.tensor_tensor(out=yc[:, :], in0=Bsb[:, 256:1280], in1=tcv[:, :],
                                                              op=ALU.mult), reads=['Bsb', 'tcv'], writes=['yc'])
                        P.op('pool', lambda e: e.tensor_tensor(out=ycsq[:, :], in0=yc[:, :], in1=yc[:, :], op=ALU.mult),
                             reads=['yc'], writes=['ycsq'])
                        P.op('pool', lambda e, cblk=cblk: e.tensor_scalar(out=ycg[:, :], in0=yc[:, :],
                                                                          scalar1=gbr[:, cblk:cblk + 1], scalar2=None,
                                                                          op0=ALU.mult),
                             reads=['yc', 'gbr'], writes=['ycg'])
                        P.dma('sp', yT_d[:, cblk, :], ycg[:, :], reads=['ycg'], writes=['yT_d%d' % cblk])
                        for n in range(2):
                            P.op('pe', lambda e, n=n: e.matmul(ps[2][:, :], lhsT=ones_b[:, :],
                                                               rhs=ycsq[:, n * 512:(n + 1) * 512], start=True, stop=True),
                                 reads=['ycsq', 'ones_b'], writes=['pb2'])
                            P.op('dve', lambda e, n=n: e.tensor_tensor(out=ssq_c[:, n * 512:(n + 1) * 512],
                                                                       in0=ps[2][:, :], in1=ssq_c[:, n * 512:(n + 1) * 512],
                                                                       op=ALU.add),
                                 reads=['pb2', 'ssq_c'], writes=['ssq_c'])
                    finish_rstd(ssq_c, 1024.0, rstd_c, 'ssq_c')
                    P.barrier()

                if stages >= 4:
                    with ExitStack() as s3:
                        KT = sb("KT", [128, 2, WIN], BF16, stack=s3)
                        QT = sb("QT", [128, 2, TOK], BF16, stack=s3)
                        Vsb = sb("Vsb", [128, NWT, 256], BF16, stack=s3)
                        tdel = sb("tdel", [128, SLEN], stack=s3)
                        tmul = sb("tmul", [128, SLEN], BF16, stack=s3)
                        kbias = sb("kbias", [128, NWT], stack=s3)
                        kfb = [sb("kfb%d" % i, [128, 256], stack=s3) for i in range(2)]
                        sqb = [sb("sqb%d" % i, [128, 256], BF16, stack=s3) for i in range(2)]
                        v1 = sb("v1", [128, 256], stack=s3)
                        v2 = sb("v2", [128, 256], stack=s3)
                        v3 = sb("v3", [128, 256], stack=s3)
                        ssb = [sb("ssb%d" % i, [128, 512], stack=s3) for i in range(4)]
                        eb = [sb("eb%d" % i, [128, 512], BF16, stack=s3) for i in range(4)]
                        pTb = [sb("pTb%d" % i, [128, 512], BF16, stack=s3) for i in range(4)]
                        rec = sb("rec", [128, 512], stack=s3)
                        yaf = sb("yaf", [128, 512], stack=s3)
                        ysq = sb("ysq", [128, 512], BF16, stack=s3)
                        yst = [sb("yst%d" % i, [128, 512], BF16, stack=s3) for i in range(2)]
                        qgs = sb("qgs", [128, 1], stack=s3)
                        kgs = sb("kgs", [128, 1], stack=s3)
                        P.dma('sp', tdel[:, :], IN("tdelta")[:, :], writes=['tdel'])
                        P.dma('sp', tmul[:, :], IN("tmult")[:, :], writes=['tmul'])
                        P.dma('sp', kbias[:, :], IN("keybias")[:, :], writes=['kbias'])
                        P.dma('sp', qgs[:, :], IN("qg")[:, :], writes=['qgs'])
                        P.dma('sp', kgs[:, :], IN("kg")[:, :], writes=['kgs'])
                        P.op('dve', lambda e: e.tensor_scalar(out=qgs[:, :], in0=qgs[:, :], scalar1=float(128 ** -0.5),
                                                              scalar2=None, op0=ALU.mult), reads=['qgs'], writes=['qgs'])
                        P.op('dve', lambda e: e.memset(ssq_a[:, :], 0.0), writes=['ssq_a'])
                        SL = slopes()

                        def qknormA(bank):
                            i = cnt['k'] % 2
                            cnt['k'] += 1
                            bkey = 'pb%d' % bank
                            P.op('act', lambda e: e.copy(out=kfb[i][:, :], in_=ps[bank][:, 0:256]),
                                 reads=[bkey], writes=['kf%d' % i])
                            P.op('pool', lambda e: e.tensor_tensor(out=sqb[i][:, :], in0=kfb[i][:, :], in1=kfb[i][:, :],
                                                                   op=ALU.mult), reads=['kf%d' % i], writes=['sqb%d' % i])
                            return i

                        def qknormB(i, dst_ap, gsc, gkey, dkey):
                            P.op('pe', lambda e: e.matmul(ps[2][:, 0:256], lhsT=ones_b[:, :], rhs=sqb[i][:, :],
                                                          start=True, stop=True),
                                 reads=['sqb%d' % i, 'ones_b'], writes=['pb2'])
                            P.op('dve', lambda e: e.tensor_scalar(out=v1[:, :], in0=ps[2][:, 0:256], scalar1=1.0 / 128,
                                                                  scalar2=EPS, op0=ALU.mult, op1=ALU.add),
                                 reads=['pb2'], writes=['v1'])
                            P.op('act', lambda e: e.activation(out=v2[:, :], in_=v1[:, :], func=AF.Sqrt),
                                 reads=['v1'], writes=['v2'])
                            P.op('dve', lambda e: e.reciprocal(out=v3[:, :], in_=v2[:, :]), reads=['v2'], writes=['v3'])
                            P.op('dve', lambda e: e.scalar_tensor_tensor(out=dst_ap, in0=kfb[i][:, :], scalar=gsc[:, 0:1],
                                                                         in1=v3[:, :], op0=ALU.mult, op1=ALU.mult),
                                 reads=['kf%d' % i, 'v3', gkey], writes=[dkey])

                        def load_pair_weights(hp):
                            h0 = 2 * hp
                            load_block(24 + h0, Wq[:, 0, :, :], 'Wq0')
                            load_block(24 + h0 + 1, Wq[:, 1, :, :], 'Wq1')
                            load_block(48 + h0, Wk[:, 0, :, :], 'Wk0')
                            load_block(48 + h0 + 1, Wk[:, 1, :, :], 'Wk1')
                            load_block(72 + h0, Wv[:, :, 0, :], 'Wv0')
                            load_block(72 + h0 + 1, Wv[:, :, 1, :], 'Wv1')

                        NHP = 12 if not LIM else 1
                        load_pair_weights(0)
                        for hp in range(NHP):
                            h0 = 2 * hp
                            for ch in range(12):
                                hb = load_hT(2 * ch)
                                ks = []
                                for hh in range(2):
                                    proj_fm(Wk[:, hh, :, :], 'Wk%d' % hh, hb, hh, 'pb%d' % hh)
                                    ks.append(qknormA(hh))
                                for t in range(2):
                                    def mmv(e, t=t, hb=hb):
                                        for kc in range(KC):
                                            r = e.matmul(ps[3][:, 0:256], lhsT=hTc[hb][:, t, kc, :], rhs=Wv[:, kc, :, :],
                                                         start=(kc == 0), stop=(kc == KC - 1))
                                        return r
                                    P.op('pe', mmv, reads=['Wv0', 'Wv1', 'hTc%d' % hb], writes=['pb3'])
                                    P.op('dve', lambda e, t=t, ch=ch: e.tensor_copy(out=Vsb[:, 2 * ch + t, :],
                                                                                    in_=ps[3][:, 0:256]),
                                         reads=['pb3'], writes=['V%d' % (2 * ch + t)])
                                for hh in range(2):
                                    qknormB(ks[hh], KT[:, hh, ch * 256:(ch + 1) * 256], kgs, 'kgs', 'KT%d_%d' % (hh, ch))
                                if ch >= 8:
                                    qs = []
                                    for hh in range(2):
                                        proj_fm(Wq[:, hh, :, :], 'Wq%d' % hh, hb, hh, 'pb%d' % hh)
                                        qs.append(qknormA(hh))
                                    for hh in range(2):
                                        qknormB(qs[hh], QT[:, hh, (ch - 8) * 256:(ch - 7) * 256], qgs, 'qgs',
                                                'QT%d_%d' % (hh, ch - 8))
                            if hp + 1 < NHP:
                                load_pair_weights(hp + 1)
                            for hh in range(2):
                                h = h0 + hh
                                for g in range(2):
                                    qb0 = 16 + 4 * g
                                    kaps = list(range(4 * g, 4 * g + 20))

                                    SB = (4, 5, 0, 1)
                                    SK = ('pS0', 'pS1', 'pb0', 'pb1')

                                    def issue_S(kap, sbk, hh=hh, g=g):
                                        P.op('pe', lambda e: e.matmul(ps[SB[sbk]][:, :], lhsT=KT[:, hh, kap * 128:(kap + 1) * 128],
                                                                      rhs=QT[:, hh, g * 512:(g + 1) * 512], start=True, stop=True),
                                             reads=['KT%d_%d' % (hh, kap // 2), 'QT%d_%d' % (hh, 2 * g), 'QT%d_%d' % (hh, 2 * g + 1)],
                                             writes=[SK[sbk]])
                                    sb0 = cnt['s']
                                    LA = 3
                                    nk = len(kaps)
                                    for a in range(LA):
                                        issue_S(kaps[a], (sb0 + a) % 4)

                                    def st_A(i):
                                        kap = kaps[i]
                                        sbk = (sb0 + i) % 4
                                        off = 128 * (qb0 - kap) + SOFF
                                        P.op('dve', lambda e: e.scalar_tensor_tensor(
                                            out=ssb[sbk][:, :], in0=tdel[:, off:off + 512], scalar=SL[h],
                                            in1=ps[SB[sbk]][:, :], op0=ALU.mult, op1=ALU.add),
                                            reads=[SK[sbk], 'tdel'], writes=['ssb%d' % sbk])

                                    def st_E(i):
                                        kap = kaps[i]
                                        sbk = (sb0 + i) % 4
                                        P.op('act', lambda e: e.activation(
                                            out=eb[sbk][:, :], in_=ssb[sbk][:, :], func=AF.Exp, bias=kbias[:, kap:kap + 1]),
                                            reads=['ssb%d' % sbk, 'kbias'], writes=['eb%d' % sbk])

                                    def st_M(i):
                                        kap = kaps[i]
                                        sbk = (sb0 + i) % 4
                                        off = 128 * (qb0 - kap) + SOFF
                                        P.op('dve', lambda e: e.tensor_tensor(
                                            out=pTb[sbk][:, :], in0=eb[sbk][:, :], in1=tmul[:, off:off + 512], op=ALU.mult),
                                            reads=['eb%d' % sbk, 'tmul'], writes=['pT%d' % sbk])

                                    def st_V(i, hh=hh):
                                        kap = kaps[i]
                                        sbk = (sb0 + i) % 4

                                        def pv(e):
                                            e.matmul(ps[6][:, :], lhsT=Vsb[:, kap, hh * 128:(hh + 1) * 128], rhs=pTb[sbk][:, :],
                                                     start=(i == 0), stop=(i == nk - 1))
                                            return e.matmul(ps[7][:, :], lhsT=ones_b[:, :], rhs=pTb[sbk][:, :],
                                                            start=(i == 0), stop=(i == nk - 1))
                                        P.op('pe', pv, reads=['pT%d' % sbk, 'V%d' % kap, 'ones_b'], writes=['pO'])

                                    for t in range(nk + 3):
                                        if t + LA < nk:
                                            issue_S(kaps[t + LA], (sb0 + t + LA) % 4)
                                        if t < nk:
                                            st_A(t)
                                        if 0 <= t - 1 < nk:
                                            st_E(t - 1)
                                        if 0 <= t - 2 < nk:
                                            st_M(t - 2)
                                        if 0 <= t - 3 < nk:
                                            st_V(t - 3)
                                    cnt['s'] = sb0 + len(kaps)
                                    P.op('dve', lambda e: e.reciprocal(out=rec[:, :], in_=ps[7][:, :]),
                                         reads=['pO'], writes=['rec'])
                                    P.op('dve', lambda e: e.tensor_tensor(out=yaf[:, :], in0=ps[6][:, :], in1=rec[:, :],
                                                                          op=ALU.mult), reads=['pO', 'rec'], writes=['yaf'])
                                    P.op('pool', lambda e: e.tensor_tensor(out=ysq[:, :], in0=yaf[:, :], in1=yaf[:, :],
                                                                           op=ALU.mult), reads=['yaf'], writes=['ysq'])
                                    P.op('pe', lambda e: e.matmul(ps[2][:, :], lhsT=ones_b[:, :], rhs=ysq[:, :],
                                                                  start=True, stop=True),
                                         reads=['ysq', 'ones_b'], writes=['pb2'])
                                    P.op('dve', lambda e, g=g: e.tensor_tensor(out=ssq_a[:, g * 512:(g + 1) * 512],
                                                                               in0=ps[2][:, :], in1=ssq_a[:, g * 512:(g + 1) * 512],
                                                                               op=ALU.add),
                                         reads=['pb2', 'ssq_a'], writes=['ssq_a'])
                                    yi = cnt['y'] % 2
                                    cnt['y'] += 1
                                    P.op('pool', lambda e, yi=yi, h=h: e.tensor_scalar(out=yst[yi][:, :], in0=yaf[:, :],
                                                                                       scalar1=gbr[:, 8 + h:9 + h], scalar2=None,
                                                                                       op0=ALU.mult),
                                         reads=['yaf', 'gbr'], writes=['yst%d' % yi])
                                    P.dma('sp', yT_d[:, 8 + h, g * 512:(g + 1) * 512], yst[yi][:, :],
                                          reads=['yst%d' % yi], writes=['yT_d%d_%d' % (8 + h, g)])
                        finish_rstd(ssq_a, 3072.0, rstd_a, 'ssq_a')
                        P.barrier()
                if dbg:
                    P.dma('sp', dbg_t[:, 0:8], rstd_c[:, :], reads=['ssq_crstd'], writes=['dbg'])
                    if stages >= 4:
                        P.dma('sp', dbg_t[:, 8:16], rstd_a[:, :], reads=['ssq_arstd'], writes=['dbg'])
                P.barrier()

        if stages >= 5:
            with ExitStack() as st:
                yT = sb("yT", [128, KC, TOK], BF16, stack=st)
                gt1b = sb("gt1b", [128, D], stack=st)
                wostg = [sb("wostg%d" % i, [128, 8, 512], stack=st) for i in range(2)]
                Wo2 = [sb("Wo%d" % i, [128, KC, 512], BF16, stack=st) for i in range(2)]
                xs = [sb("xs%d" % i, [128, 512], stack=st) for i in range(2)]
                t1 = [sb("t1_%d" % i, [128, 512], stack=st) for i in range(2)]
                x2s = [sb("x2s%d" % i, [128, 512], stack=st) for i in range(2)]
                for k8 in range(4):
                    P.dma('sp', yT[:, k8 * 8:(k8 + 1) * 8, :], yT_d[:, k8 * 8:(k8 + 1) * 8, :], writes=['yT%d' % k8])
                P.dma('sp', gt1b[:, :], gtb_d[0, :, :], writes=['gt1b'])
                nw = 0
                nt = 0
                def load_wo(cb):
                    nonlocal_nw = cnt.setdefault('wo', 0)
                    for k8 in range(4):
                        i = cnt['wo'] % 2
                        cnt['wo'] += 1
                        P.dma('sp', wostg[i][:, :, :],
                              IN("w_out")[k8 * 1024:(k8 + 1) * 1024, cb * 512:(cb + 1) * 512].rearrange('(k p) c -> p k c', p=128),
                              writes=['wostg%d' % i])
                        dst = Wo2[cb % 2][:, k8 * 8:(k8 + 1) * 8, :]
                        if i == 0:
                            P.op('pool', lambda e, dst=dst, i=i: e.tensor_copy(out=dst, in_=wostg[i][:, :, :]),
                                 reads=['wostg%d' % i], writes=['Wo%d_%d' % (cb % 2, k8)])
                        else:
                            P.op('act', lambda e, dst=dst, i=i: e.copy(out=dst, in_=wostg[i][:, :, :]),
                                 reads=['wostg%d' % i], writes=['Wo%d_%d' % (cb % 2, k8)])
                load_wo(0)
                for cb in range(8):
                    if cb + 1 < 8:
                        load_wo(cb + 1)
                    Wo = Wo2[cb % 2]
                    wkeys = ['Wo%d_%d' % (cb % 2, k8) for k8 in range(4)]
                    for tile in range(8):
                        j = nt % 2
                        nt += 1
                        bc, ba = (0, 1) if j == 0 else (2, 3)

                        def mmo(e, tile=tile, bc=bc, ba=ba, Wo=Wo):
                            for kc in range(8):
                                e.matmul(ps[bc][:, :], lhsT=yT[:, kc, tile * 128:(tile + 1) * 128], rhs=Wo[:, kc, :],
                                         start=(kc == 0), stop=(kc == 7))
                            for kc in range(8, KC):
                                r = e.matmul(ps[ba][:, :], lhsT=yT[:, kc, tile * 128:(tile + 1) * 128], rhs=Wo[:, kc, :],
                                             start=(kc == 8), stop=(kc == KC - 1))
                            return r
                        P.op('pe', mmo, reads=['yT0', 'yT1', 'yT2', 'yT3'] + wkeys, writes=['po%d' % j])
                        P.dma('sp', xs[j][:, :], IN("xw")[HALO + tile * 128:HALO + (tile + 1) * 128, cb * 512:(cb + 1) * 512],
                              writes=['xs%d' % j])
                        P.op('dve', lambda e, j=j, tile=tile, bc=bc: e.tensor_scalar(
                            out=t1[j][:, :], in0=ps[bc][:, :], scalar1=rstd_c[:, tile:tile + 1], scalar2=None, op0=ALU.mult),
                            reads=['po%d' % j], writes=['t1_%d' % j])
                        P.op('dve', lambda e, j=j, tile=tile, ba=ba: e.scalar_tensor_tensor(
                            out=t1[j][:, :], in0=ps[ba][:, :], scalar=rstd_a[:, tile:tile + 1], in1=t1[j][:, :],
                            op0=ALU.mult, op1=ALU.add), reads=['po%d' % j, 't1_%d' % j], writes=['t1_%d' % j])
                        P.op('dve', lambda e, j=j, cb=cb: e.tensor_tensor(
                            out=t1[j][:, :], in0=t1[j][:, :], in1=gt1b[:, cb * 512:(cb + 1) * 512], op=ALU.mult),
                            reads=['t1_%d' % j, 'gt1b'], writes=['t1_%d' % j])
                        P.op('dve', lambda e, j=j: e.tensor_tensor(out=x2s[j][:, :], in0=t1[j][:, :], in1=xs[j][:, :], op=ALU.add),
                             reads=['t1_%d' % j, 'xs%d' % j], writes=['x2s%d' % j])
                        P.dma('sp', out[tile * 128:(tile + 1) * 128, cb * 512:(cb + 1) * 512], x2s[j][:, :],
                              reads=['x2s%d' % j], writes=['out_%d_%d' % (tile, cb)])
                P.barrier()
            P.barrier()

        if stages >= 6:
            with ExitStack() as st:
                h2T = sb("h2T", [128, KC, TOK], BF16, stack=st)
                with ExitStack() as s5:
                    xt5 = sb("xt5", [128, D], stack=s5)
                    junk5 = sb("junk5", [128, D], BF16, stack=s5)
                    ss5 = sb("ss5", [128, 1], stack=s5)
                    rs5 = sb("rs5", [128, 2], stack=s5)
                    hTf = sb("hTf", [128, KC, 128], stack=s5)
                    wr = sb("wr", [128, KC, 72], stack=s5)
                    L = sb("L", [128, 72], stack=s5)
                    sm = sb("sm", [128, 16], stack=s5)
                    goh = sb("goh", [128, 8], stack=s5)
                    gex = sb("gex", [128, 8], stack=s5)
                    ein = sb("ein", [128, 8], stack=s5)
                    e2 = sb("e2", [128, 8], stack=s5)
                    oh1 = sb("oh1", [128, 8], stack=s5)
                    oh2 = sb("oh2", [128, 8], stack=s5)
                    wsel = sb("wsel", [128, 8], stack=s5)
                    wgt = sb("wgt", [128, 64], stack=s5)
                    wgtTs = sb("wgtTs", [64, 128], stack=s5)
                    P.dma('sp', wr[:, :, :].rearrange('p k c -> p (k c)'), IN("w_r")[:, :], writes=['wr'])
                    for tile in range(8):
                        key = 's5_'
                        norm_transpose(out[tile * 128:(tile + 1) * 128, :], xt5, junk5, ss5, rs5,
                                       (lambda kc, tile=tile: h2T[:, kc, tile * 128:(tile + 1) * 128]),
                                       A2, B2, key, (0, 1), hTf=hTf)

                        def mml(e):
                            for kc in range(KC):
                                r = e.matmul(ps[2][:, 0:72], lhsT=hTf[:, kc, :], rhs=wr[:, kc, :],
                                             start=(kc == 0), stop=(kc == KC - 1))
                            return r
                        P.op('pe', mml, reads=['wr'] + [key + 'hT%df' % kc for kc in range(KC)], writes=['pb2'])
                        V = lambda e: e
                        P.op('dve', lambda e: e.tensor_copy(out=L[:, :], in_=ps[2][:, 0:72]), reads=['pb2'], writes=['R'])
                        P.op('dve', lambda e: e.tensor_reduce(out=sm[:, 0:1], in_=L[:, 0:8], axis=AX.X, op=ALU.max),
                             reads=['R'], writes=['R'])
                        P.op('dve', lambda e: e.tensor_scalar(out=goh[:, :], in0=L[:, 0:8], scalar1=sm[:, 0:1], scalar2=None,
                                                              op0=ALU.is_equal), reads=['R'], writes=['R'])
                        P.op('dve', lambda e: e.tensor_scalar(out=sm[:, 1:2], in0=sm[:, 0:1], scalar1=-1.0, scalar2=None,
                                                              op0=ALU.mult), reads=['R'], writes=['R'])
                        P.op('dve', lambda e: e.memset(sm[:, 2:3], 0.0), reads=['R'], writes=['R'])
                        P.op('act', lambda e: e.activation(out=gex[:, :], in_=L[:, 0:8], func=AF.Exp, bias=sm[:, 1:2],
                                                           accum_out=sm[:, 2:3]), reads=['R'], writes=['R'])
                        P.op('dve', lambda e: e.reciprocal(out=sm[:, 3:4], in_=sm[:, 2:3]), reads=['R'], writes=['R'])
                        P.op('dve', lambda e: e.tensor_scalar(out=ein[:, :], in0=L[:, 8:16], scalar1=goh[:, 0:1], scalar2=None,
                                                              op0=ALU.mult), reads=['R'], writes=['R'])
                        for g in range(1, 8):
                            P.op('dve', lambda e, g=g: e.scalar_tensor_tensor(
                                out=ein[:, :], in0=L[:, 8 + 8 * g:16 + 8 * g], scalar=goh[:, g:g + 1], in1=ein[:, :],
                                op0=ALU.mult, op1=ALU.add), reads=['R'], writes=['R'])
                        P.op('dve', lambda e: e.tensor_reduce(out=sm[:, 4:5], in_=ein[:, :], axis=AX.X, op=ALU.max),
                             reads=['R'], writes=['R'])
                        P.op('dve', lambda e: e.tensor_scalar(out=oh1[:, :], in0=ein[:, :], scalar1=sm[:, 4:5], scalar2=None,
                                                              op0=ALU.is_equal), reads=['R'], writes=['R'])
                        P.op('dve', lambda e: e.scalar_tensor_tensor(out=e2[:, :], in0=oh1[:, :], scalar=-1.0e30, in1=ein[:, :],
                                                                     op0=ALU.mult, op1=ALU.add), reads=['R'], writes=['R'])
                        P.op('dve', lambda e: e.tensor_reduce(out=sm[:, 5:6], in_=e2[:, :], axis=AX.X, op=ALU.max),
                             reads=['R'], writes=['R'])
                        P.op('dve', lambda e: e.tensor_scalar(out=oh2[:, :], in0=e2[:, :], scalar1=sm[:, 5:6], scalar2=None,
                                                              op0=ALU.is_equal), reads=['R'], writes=['R'])
                        P.op('dve', lambda e: e.tensor_tensor(out=sm[:, 6:7], in0=sm[:, 5:6], in1=sm[:, 4:5], op=ALU.subtract),
                             reads=['R'], writes=['R'])
                        P.op('act', lambda e: e.activation(out=sm[:, 7:8], in_=sm[:, 6:7], func=AF.Exp), reads=['R'], writes=['R'])
                        P.op('dve', lambda e: e.tensor_scalar(out=sm[:, 8:9], in0=sm[:, 7:8], scalar1=1.0, scalar2=None,
                                                              op0=ALU.add), reads=['R'], writes=['R'])
                        P.op('dve', lambda e: e.reciprocal(out=sm[:, 9:10], in_=sm[:, 8:9]), reads=['R'], writes=['R'])
                        P.op('dve', lambda e: e.tensor_tensor(out=sm[:, 10:11], in0=sm[:, 7:8], in1=sm[:, 9:10], op=ALU.mult),
                             reads=['R'], writes=['R'])
                        P.op('dve', lambda e: e.tensor_scalar(out=sm[:, 9:11], in0=sm[:, 9:11], scalar1=sm[:, 3:4], scalar2=None,
                                                              op0=ALU.mult), reads=['R'], writes=['R'])
                        P.op('dve', lambda e: e.tensor_scalar(out=wsel[:, :], in0=oh1[:, :], scalar1=sm[:, 9:10], scalar2=None,
                                                              op0=ALU.mult), reads=['R'], writes=['R'])
                        P.op('dve', lambda e: e.scalar_tensor_tensor(out=wsel[:, :], in0=oh2[:, :], scalar=sm[:, 10:11],
                                                                     in1=wsel[:, :], op0=ALU.mult, op1=ALU.add),
                             reads=['R'], writes=['R'])
                        for g in range(8):
                            P.op('dve', lambda e, g=g: e.tensor_scalar(out=wgt[:, 8 * g:8 * g + 8], in0=wsel[:, :],
                                                                       scalar1=goh[:, g:g + 1], scalar2=None, op0=ALU.mult),
                                 reads=['R', 'wgtrd'], writes=['R'])
                        P.op('pe', lambda e: e.transpose(out=ps[3][0:64, 0:128], in_=wgt[:, 0:64], identity=ident[:, :]),
                             reads=['R', 'ident'], writes=['pb3', 'wgtrd'])
                        P.op('dve', lambda e: e.tensor_copy(out=wgtTs[:, :], in_=ps[3][0:64, 0:128]),
                             reads=['pb3'], writes=['wgtTs'])
                        P.dma('sp', wgtT_d[:, tile * 128:(tile + 1) * 128], wgtTs[:, :], reads=['wgtTs'], writes=['wgtT_d'])
                    P.barrier()
                if stages >= 7:
                    with ExitStack() as s6:
                        wstg6 = [sb("wstg6_%d" % i, [128, (KC // 2) * 128], stack=s6) for i in range(4)]
                        W13 = [[sb("W13_%d_%d" % (a, i), [128, KC, 128], BF16, stack=s6) for i in range(2)] for a in range(2)]
                        actT = sb("actT", [128, 2, 4, TOK], BF16, stack=s6)
                        wb = [sb("wb%d" % i, [128, TOK], stack=s6) for i in range(2)]
                        s1 = [sb("s1_%d" % i, [128, 512], stack=s6) for i in range(2)]
                        w2stg = [sb("w2stg%d" % i, [128, 4 * 512], stack=s6) for i in range(2)]
                        W2b = [sb("W2b%d" % i, [128, 2, 4, 512], BF16, stack=s6) for i in range(2)]
                        gt2s = [sb("gt2s%d" % i, [128, 512], stack=s6) for i in range(2)]
                        NOST = 6
                        ost = [sb("ost%d" % i, [128, 512], stack=s6) for i in range(NOST)]
                        NP = NEXP // 2 if not LIM else 1
                        steps = []
                        na = 0
                        nb = 0
                        for pair in range(NP):
                            for el in range(2):
                                for j in range(4):
                                    steps.append(('A', pair, el, j, na % 2))
                                    na += 1
                            for cb in range(8):
                                steps.append(('B', pair, cb, nb % 2))
                                nb += 1
                        cn = {'l': 0, 'w2': 0, 'o': 0}

                        def cast3(i, dst, src3, skey, dkey, engs):
                            eng = engs[i]
                            if eng == 'act':
                                P.op('act', lambda e: e.copy(out=dst, in_=src3), reads=[skey], writes=[dkey])
                            else:
                                P.op(eng, lambda e: e.tensor_copy(out=dst, in_=src3), reads=[skey], writes=[dkey])

                        def prefetch(stp):
                            if stp[0] == 'A':
                                _, pair, el, j, a = stp
                                ex = pair * 2 + el
                                if j == 0:
                                    P.dma('sp', wb[el][:, :], wgtT_d[ex:ex + 1, :].to_broadcast([128, TOK]), writes=['wb%d' % el])
                                for wi, wname in enumerate(("w1_t", "w3_t")):
                                    for hf in range(2):
                                        i = cn['l'] % 4
                                        cn['l'] += 1
                                        P.dma('sp', wstg6[i][:, :], IN(wname)[ex, j, :, hf * 2048:(hf + 1) * 2048], writes=['wstg6_%d' % i])
                                        src3 = wstg6[i][:, :].rearrange('p (k c) -> p k c', k=KC // 2)
                                        cast3((wi + hf) % 2, W13[a][wi][:, hf * 16:(hf + 1) * 16, :], src3, 'wstg6_%d' % i,
                                              'W13_%d_%d_%d' % (a, wi, hf), ('dve', 'act'))
                            else:
                                _, pair, cb, wa_ = stp
                                for el in range(2):
                                    ex = pair * 2 + el
                                    i = cn['w2'] % 2
                                    cn['w2'] += 1
                                    P.dma('sp', w2stg[i][:, :], IN("w2_t")[ex, cb, :, :], writes=['w2stg%d' % i])
                                    src3 = w2stg[i][:, :].rearrange('p (k c) -> p k c', k=4)
                                    cast3(i, W2b[wa_][:, el, :, :], src3, 'w2stg%d' % i, 'W2b%d_%d' % (wa_, el), ('pool', 'act'))
                                P.dma('sp', gt2s[wa_][:, :], gtb_d[1, :, cb * 512:(cb + 1) * 512], writes=['gt2s%d' % wa_])

                        def compute(stp):
                            if stp[0] == 'A':
                                _, pair, el, j, a = stp

                                for chk in range(2):
                                    def mm13(e, chk=chk):
                                        for wi in range(2):
                                            for kc in range(KC):
                                                r = e.matmul(ps[2 * chk + wi][:, :], lhsT=W13[a][wi][:, kc, :],
                                                             rhs=h2T[:, kc, chk * 512:(chk + 1) * 512],
                                                             start=(kc == 0), stop=(kc == KC - 1))
                                        return r
                                    P.op('pe', mm13, reads=['W13_%d_%d_%d' % (a, wi_, hf_) for wi_ in range(2) for hf_ in range(2)] + ['h2T'], writes=['pa%d' % chk])
                                for chk in range(2):
                                    P.op('act', lambda e, chk=chk: e.activation(out=s1[chk][:, :], in_=ps[2 * chk][:, :], func=AF.Silu),
                                         reads=['pa%d' % chk], writes=['s1_%d' % chk])
                                    P.op('dve', lambda e, chk=chk: e.tensor_tensor(out=s1[chk][:, :], in0=ps[2 * chk + 1][:, :],
                                                                                  in1=s1[chk][:, :], op=ALU.mult),
                                         reads=['pa%d' % chk, 's1_%d' % chk], writes=['s1_%d' % chk])
                                    P.op('pool', lambda e, chk=chk: e.tensor_tensor(
                                        out=actT[:, el, j, chk * 512:(chk + 1) * 512], in0=s1[chk][:, :],
                                        in1=wb[el][:, chk * 512:(chk + 1) * 512], op=ALU.mult),
                                        reads=['s1_%d' % chk, 'wb%d' % el], writes=['actT'])
                            else:
                                _, pair, cb, wa_ = stp
                                for tile in range(8):
                                    o = cn['o'] % 2
                                    ob = cn['o'] % NOST
                                    cn['o'] += 1

                                    def mm2(e, tile=tile, o=o):
                                        n = 0
                                        for el in range(2):
                                            for kc in range(4):
                                                r = e.matmul(ps[4 + o][:, :], lhsT=actT[:, el, kc, tile * 128:(tile + 1) * 128],
                                                             rhs=W2b[wa_][:, el, kc, :], start=(n == 0), stop=(n == 7))
                                                n += 1
                                        return r
                                    P.op('pe', mm2, reads=['actT', 'W2b%d_0' % wa_, 'W2b%d_1' % wa_], writes=['pm%d' % o])
                                    P.op('dve', lambda e, o=o, ob=ob: e.tensor_tensor(out=ost[ob][:, :], in0=ps[4 + o][:, :],
                                                                                     in1=gt2s[wa_][:, :], op=ALU.mult),
                                         reads=['pm%d' % o, 'gt2s%d' % wa_], writes=['ost%d' % ob])
                                    P.dma('pool', out[tile * 128:(tile + 1) * 128, cb * 512:(cb + 1) * 512], ost[ob][:, :],
                                          reads=['ost%d' % ob], writes=['out_%d_%d' % (tile, cb)], accum_op=ALU.add)

                        prefetch(steps[0])
                        for si, stp in enumerate(steps):
                            if si + 1 < len(steps):
                                prefetch(steps[si + 1])
                            compute(stp)
                        P.barrier()
            P.barrier()

        P.barrier()
    return nc, sorted(_in)


def _mult(delta):
    d = np.asarray(delta)
    m = ((d >= 0) & (d <= 128)).astype(np.float32)
    m += ((d >= 0) & (d <= 512) & (d % 4 == 0)).astype(np.float32)
    m += ((d >= 0) & (d <= 2048) & (d % 16 == 0)).astype(np.float32)
    return m


def host_consts():
    import ml_dtypes
    u = np.arange(128)[:, None]
    cidx = np.arange(SLEN)[None, :]
    delta = cidx - SOFF - u
    tdelta = (-np.maximum(delta, 0)).astype(np.float32)
    tmult = _mult(delta).astype(ml_dtypes.bfloat16)
    return {"ident": np.eye(128, dtype=np.float32), "tdelta": np.ascontiguousarray(tdelta),
            "tmult": np.ascontiguousarray(tmult)}


def t32(v):
    return np.ascontiguousarray(np.asarray(v, np.float32).reshape(KC, 128).T)


def host_shared(inp, need):
    sh = {}
    if "c_t" in need:
        sh["c_t"] = t32(inp["c"][0])
    if "w_ada" in need:
        sh["w_ada"] = np.ascontiguousarray(inp["w_ada"][0])
    if "b_ada" in need:
        sh["b_ada"] = np.ascontiguousarray(inp["b_ada"][0][None, :])
    if "g_mix_t" in need:
        sh["g_mix_t"] = t32(inp["g_mix"][0])
    if "g_ffn_t" in need:
        sh["g_ffn_t"] = t32(inp["g_ffn"][0])
    if "g_br_t" in need:
        sh["g_br_t"] = t32(inp["g_branch"][0])
    if "w_in_t" in need:
        w = inp["w_in"][0].reshape(KC, 128, 96, 128).transpose(2, 1, 0, 3)
        sh["w_in_t"] = np.ascontiguousarray(w).reshape(96, 128, KC * 128)
    if "conv_w_t" in need:
        cw = inp["conv_w"][0].reshape(3, 8, 128).transpose(2, 1, 0)
        sh["conv_w_t"] = np.ascontiguousarray(cw).reshape(128, 24)
    if "qg" in need:
        sh["qg"] = np.ascontiguousarray(inp["q_norm_g"][0][:, None])
    if "kg" in need:
        sh["kg"] = np.ascontiguousarray(inp["k_norm_g"][0][:, None])
    if "w_out" in need:
        sh["w_out"] = np.ascontiguousarray(inp["w_out"][0])
    if "w_r" in need:
        wr = np.concatenate([inp["w_group"][0], inp["w_expert"][0]], axis=1)
        sh["w_r"] = np.ascontiguousarray(wr.reshape(KC, 128, 72).transpose(1, 0, 2)).reshape(128, KC * 72)
    for nm, src in (("w1_t", "w1"), ("w3_t", "w3")):
        if nm in need:
            w = inp[src][0].reshape(NEXP, KC, 128, 4, 128).transpose(0, 3, 2, 1, 4)
            sh[nm] = np.ascontiguousarray(w).reshape(NEXP, 4, 128, KC * 128)
    if "w2_t" in need:
        w = inp["w2"][0].reshape(NEXP, 4, 128, 8, 512).transpose(0, 3, 2, 1, 4)
        sh["w2_t"] = np.ascontiguousarray(w).reshape(NEXP, 8, 128, 4 * 512)
    hc = host_consts()
    for k in hc:
        if k in need:
            sh[k] = hc[k]
    return sh


def host_core(inp, core, need):
    m = {}
    t0 = core * TOK - HALO
    if "xw" in need:
        x = inp["x"][0]
        xw = np.zeros((WIN, D), np.float32)
        lo = max(t0, 0)
        xw[lo - t0:] = x[lo:t0 + WIN]
        m["xw"] = xw
    if "keybias" in need:
        pos = t0 + np.arange(WIN)
        kb = np.where(pos >= 0, 0.0, NEG).astype(np.float32)
        m["keybias"] = np.ascontiguousarray(kb.reshape(NWT, 128).T)
    if "cmask" in need:
        m["cmask"] = np.full((128, 2), 1.0 if core > 0 else 0.0, np.float32)
    return m


_CACHE = {}


def kernel(**inputs):
    if "nc" not in _CACHE:
        _CACHE["nc"] = build()
    nc, need = _CACHE["nc"]
    sh = host_shared(inputs, need)
    in_maps = []
    for c in range(NCORES):
        m = dict(sh)
        m.update(host_core(inputs, c, need))
        in_maps.append(m)
    res = run_bass_kernel_spmd(nc, in_maps, core_ids=list(range(NCORES)))
    return np.concatenate([np.asarray(r["out"]) for r in res.results], axis=0).reshape(1, S, D).astype(np.float32)
```

```python
import numpy as np
from contextlib import ExitStack
import concourse.bass as bass
import concourse.mybir as mybir
from concourse.bass_utils import run_bass_kernel_spmd

F32 = mybir.dt.float32
BF16 = mybir.dt.bfloat16
ALU = mybir.AluOpType
AF = mybir.ActivationFunctionType
AX = mybir.AxisListType

NCORES = 8
D = 4096
S = 8192
TOK = S // NCORES
HALO = 2048
WIN = TOK + HALO
NWT = WIN // 128
KC = D // 128
NH = 24
EPS = 1e-6
NEG = -30000.0
SOFF = 384
SLEN = SOFF + 2048 + 512
NEXP = 64
import os
LIM = int(os.environ.get('KLIM', '0'))
SKIP = os.environ.get('KSKIP', '')
LIMM = int(os.environ.get('KLIMM', '0'))


class Prog:
    NQ = 10

    def __init__(self, nc, es):
        self.nc = nc
        self.engs = {'pe': nc.tensor, 'dve': nc.vector, 'act': nc.scalar,
                     'pool': nc.gpsimd, 'sp': nc.sync}
        self.sems = []
        self.csem = {}
        self.ccnt = {}
        for e in ['pe', 'dve', 'act', 'pool']:
            self.csem[e] = self._newsem(es, 'c_' + e)
            self.ccnt[e] = 0
        self.dsem = {}
        self.dcnt = {}
        self.dnext = {}
        for q in ['sp', 'act', 'pool']:
            self.dsem[q] = [self._newsem(es, 'd_%s%d' % (q, i)) for i in range(self.NQ)]
            self.dcnt[q] = [0] * self.NQ
            self.dnext[q] = 0
        self.seen = {e: {} for e in self.engs}
        self.lastw = {}
        self.readers = {}
        self.nins = 0
        self.pe_pending_wait = False
        self.pe_dummy = None

    def _newsem(self, es, name):
        h = es.enter_context(self.nc.semaphore(name))
        self.sems.append(h)
        return len(self.sems) - 1

    def _wait(self, e, tok):
        si, val = tok
        if e == 'pe' and si == self.csem['pe']:
            return
        if self.seen[e].get(si, 0) >= val:
            return
        if e == 'pe' and self.pe_pending_wait and self.pe_dummy is not None:
            self.engs[e].ldweights(self.pe_dummy)
        self.engs[e].wait_ge(self.sems[si], val)
        if e == 'pe':
            self.pe_pending_wait = True
        self.seen[e][si] = val

    def _deps(self, reads, writes):
        deps = {}
        def add(tok):
            if tok is None:
                return
            si, v = tok
            if deps.get(si, 0) < v:
                deps[si] = v
        for k in reads:
            add(self.lastw.get(k))
        for k in writes:
            add(self.lastw.get(k))
            for t in self.readers.get(k, ()):
                add(t)
        return list(deps.items())

    def _record(self, tok, reads, writes):
        for k in reads:
            self.readers.setdefault(k, []).append(tok)
        for k in writes:
            self.lastw[k] = tok
            self.readers[k] = []

    def op(self, e, fn, reads=(), writes=()):
        for tok in self._deps(reads, writes):
            self._wait(e, tok)
        ins = fn(self.engs[e])
        if e == 'pe':
            self.pe_pending_wait = False
        self.ccnt[e] += 1
        ins.then_inc(self.sems[self.csem[e]], 1)
        self._record((self.csem[e], self.ccnt[e]), reads, writes)
        self.nins += 1

    def dma(self, q, out, in_, reads=(), writes=(), **kw):
        i = self.dnext[q]
        self.dnext[q] = (i + 1) % self.NQ
        si = self.dsem[q][i]
        if self.dcnt[q][i]:
            self._wait(q, (si, self.dcnt[q][i]))
        for tok in self._deps(reads, writes):
            self._wait(q, tok)
        ins = self.engs[q].dma_start(out=out, in_=in_, **kw)
        self.dcnt[q][i] += 16
        ins.then_inc(self.sems[si], 16)
        self._record((si, self.dcnt[q][i]), reads, writes)
        self.nins += 1

    def barrier(self):
        toks = []
        for e in self.csem:
            if self.ccnt[e]:
                toks.append((self.csem[e], self.ccnt[e]))
        for q in self.dsem:
            for i in range(self.NQ):
                if self.dcnt[q][i]:
                    toks.append((self.dsem[q][i], self.dcnt[q][i]))
        for e in self.engs:
            for t in toks:
                if e in self.csem and t[0] == self.csem[e]:
                    continue
                self._wait(e, t)
        self.lastw = {}
        self.readers = {}


def slopes():
    return [float(2.0 ** (-8.0 * (h + 1) / NH)) for h in range(NH)]


def build(stages=99, dbg=None):
    nc = bass.Bass("TRN2", target_bir_lowering=False)

    def din(name, shape, dt=F32):
        return nc.dram_tensor(name, list(shape), dt, kind="ExternalInput").ap()

    dbg = set(dbg or ())

    INSHAPES = {
        "xw": ([WIN, D], F32), "c_t": ([128, KC], F32), "w_ada": ([D, 6 * D], F32),
        "b_ada": ([1, 6 * D], F32), "g_mix_t": ([128, KC], F32), "g_ffn_t": ([128, KC], F32),
        "g_br_t": ([128, KC], F32), "w_in_t": ([96, 128, KC * 128], F32),
        "conv_w_t": ([128, 24], F32), "qg": ([128, 1], F32), "kg": ([128, 1], F32),
        "w_out": ([D, D], F32), "w_r": ([128, KC * 72], F32),
        "w1_t": ([NEXP, 4, 128, KC * 128], F32), "w3_t": ([NEXP, 4, 128, KC * 128], F32),
        "w2_t": ([NEXP, 8, 128, 4 * 512], F32), "ident": ([128, 128], F32),
        "tdelta": ([128, SLEN], F32), "tmult": ([128, SLEN], BF16),
        "keybias": ([128, NWT], F32), "cmask": ([128, 2], F32),
    }
    _in = {}

    def IN(name):
        if name not in _in:
            shp, dt = INSHAPES[name]
            _in[name] = din(name, shp, dt)
        return _in[name]

    out = nc.dram_tensor("out", [TOK, D], F32, kind="ExternalOutput").ap()
    dbg_t = None
    if dbg:
        dbg_t = nc.dram_tensor("dbg", [128, 256], F32, kind="ExternalOutput").ap()

    def dscr(name, shape, dt):
        kind = "ExternalOutput" if name in dbg else "Internal"
        return nc.dram_tensor(name, list(shape), dt, kind=kind).ap()

    hT_d = dscr("hT_d", [NWT, 128, KC * 128], BF16)
    yT_d = dscr("yT_d", [128, KC, TOK], BF16)
    gtb_d = dscr("gtb_d", [2, 128, D], F32)
    wgtT_d = dscr("wgtT_d", [NEXP, TOK], F32)

    es = ExitStack()
    with es:
        P = Prog(nc, es)

        def sb(name, shape, dt=F32, stack=es):
            return stack.enter_context(nc.sbuf_tensor(name, list(shape), dt))

        ps = [es.enter_context(nc.psum_tensor("ps%d" % i, [128, 512], F32)) for i in range(8)]

        ident = sb("ident_sb", [128, 128])
        ones_f = sb("ones_f", [128, 128])
        ones_b = sb("ones_b", [128, 128], BF16)
        modT = sb("modT", [128, 192])
        A1 = sb("A1", [128, KC])
        A2 = sb("A2", [128, KC])
        gbr = sb("gbr", [128, KC])
        tmp32 = sb("tmp32", [128, KC])
        rstd_c = sb("rstd_c", [128, 8])
        rstd_a = sb("rstd_a", [128, 8])

        P.dma('sp', ident[:, :], IN("ident")[:, :], writes=['ident'])
        P.op('dve', lambda e: e.memset(ones_f[:, :], 1.0), writes=['ones_f'])
        P.op('dve', lambda e: e.memset(ones_b[:, :], 1.0), writes=['ones_b'])
        P.dma('sp', gbr[:, :], IN("g_br_t")[:, :], writes=['gbr'])

        def B1(kc):
            return modT[:, 0 * KC + kc:0 * KC + kc + 1]

        def B2(kc):
            return modT[:, 3 * KC + kc:3 * KC + kc + 1]

        with ExitStack() as st:
            c_sb = sb("c_sb", [128, KC], stack=st)
            sc = sb("sc", [128, KC], stack=st)
            wa = [sb("wa%d" % i, [128, 2048], stack=st) for i in range(3)]
            brow = sb("brow", [1, 2048], stack=st)
            row = sb("row", [1, 2048], stack=st)
            gstage = sb("gstage", [128, 2048], stack=st)
            gm = sb("gm", [128, KC], stack=st)
            gf = sb("gf", [128, KC], stack=st)

            P.dma('sp', c_sb[:, :], IN("c_t")[:, :], writes=['c_sb'])
            P.dma('sp', gm[:, :], IN("g_mix_t")[:, :], writes=['gm'])
            P.dma('sp', gf[:, :], IN("g_ffn_t")[:, :], writes=['gf'])
            P.op('act', lambda e: e.activation(out=sc[:, :], in_=c_sb[:, :], func=AF.Silu),
                 reads=['c_sb'], writes=['sc'])
            nd = 0
            for cb2 in (range(12) if not LIMM else (0, 4)):
                c0 = cb2 * 2048
                P.dma('sp', brow[0:1, :], IN("b_ada")[0:1, c0:c0 + 2048], writes=['brow'])
                for kc in range(KC):
                    buf = nd % 3
                    nd += 1
                    P.dma('sp', wa[buf][:, :], IN("w_ada")[kc * 128:(kc + 1) * 128, c0:c0 + 2048],
                          writes=['wa%d' % buf])

                    def mm(e, kc=kc, buf=buf):
                        for n in range(4):
                            r = e.matmul(ps[n][0:1, :], lhsT=sc[:, kc:kc + 1],
                                         rhs=wa[buf][:, n * 512:(n + 1) * 512],
                                         start=(kc == 0), stop=(kc == KC - 1))
                        return r
                    P.op('pe', mm, reads=['sc', 'wa%d' % buf], writes=['psM'])
                for n in range(4):
                    P.op('dve', lambda e, n=n: e.tensor_tensor(
                        out=row[0:1, n * 512:(n + 1) * 512], in0=ps[n][0:1, :],
                        in1=brow[0:1, n * 512:(n + 1) * 512], op=ALU.add),
                        reads=['psM', 'brow'], writes=['row'])

                def tr(e, cb2=cb2):
                    for q in range(16):
                        j = cb2 * 16 + q
                        r = e.matmul(ps[4][:, j:j + 1], lhsT=row[0:1, q * 128:(q + 1) * 128],
                                     rhs=ones_f[0:1, 0:1], start=True, stop=True)
                    return r
                P.op('pe', tr, reads=['row', 'ones_f'], writes=['psT'])
                if cb2 in (4, 5, 10, 11):
                    which = 0 if cb2 < 6 else 1
                    gc0 = (cb2 % 2) * 2048

                    def bc(e):
                        for n in range(4):
                            r = e.matmul(ps[n][:, :], lhsT=ones_f[0:1, :],
                                         rhs=row[0:1, n * 512:(n + 1) * 512], start=True, stop=True)
                        return r
                    P.op('pe', bc, reads=['row', 'ones_f'], writes=['psM'])
                    for n in range(4):
                        P.op('act', lambda e, n=n: e.copy(out=gstage[:, n * 512:(n + 1) * 512],
                                                          in_=ps[n][:, :]),
                             reads=['psM'], writes=['gstage'])
                    P.dma('sp', gtb_d[which, :, gc0:gc0 + 2048], gstage[:, :],
                          reads=['gstage'], writes=['gtb_d'])
            if LIMM:
                P.op('dve', lambda e: e.memset(ps[4][:, 0:192], 0.5), writes=['psT'])
            P.op('dve', lambda e: e.tensor_copy(out=modT[:, :], in_=ps[4][:, 0:192]),
                 reads=['psT'], writes=['modT'])
            P.op('dve', lambda e: e.tensor_scalar(out=tmp32[:, :], in0=modT[:, KC:2 * KC], scalar1=1.0,
                                                  scalar2=None, op0=ALU.add),
                 reads=['modT'], writes=['tmp32'])
            P.op('dve', lambda e: e.tensor_tensor(out=A1[:, :], in0=tmp32[:, :], in1=gm[:, :], op=ALU.mult),
                 reads=['tmp32', 'gm'], writes=['A1'])
            P.op('dve', lambda e: e.tensor_scalar(out=tmp32[:, :], in0=modT[:, 4 * KC:5 * KC], scalar1=1.0,
                                                  scalar2=None, op0=ALU.add),
                 reads=['modT', 'A1'], writes=['tmp32'])
            P.op('dve', lambda e: e.tensor_tensor(out=A2[:, :], in0=tmp32[:, :], in1=gf[:, :], op=ALU.mult),
                 reads=['tmp32', 'gf'], writes=['A2'])
            P.barrier()

        if dbg:
            P.dma('sp', dbg_t[:, 0:192], modT[:, :], reads=['modT'], writes=['dbg'])
            P.dma('sp', dbg_t[:, 192:224], A1[:, :], reads=['A1'], writes=['dbg'])
            P.dma('sp', dbg_t[:, 224:256], A2[:, :], reads=['A2'], writes=['dbg'])

        def norm_transpose(src_ap, xt, junk, ss, rs, hTdst, Aap, Bfn, key, pb, hTf=None):
            P.dma('sp', xt[:, :], src_ap, reads=[key + 'src'], writes=[key + 'xt'])
            P.op('dve', lambda e: e.memset(ss[:, :], 0.0), writes=[key + 'ss'])
            P.op('act', lambda e: e.activation(out=junk[:, :], in_=xt[:, :], func=AF.Square,
                                               accum_out=ss[:, 0:1]),
                 reads=[key + 'xt', key + 'ss'], writes=[key + 'junk', key + 'ss'])
            P.op('act', lambda e: e.activation(out=rs[:, 0:1], in_=ss[:, 0:1], func=AF.Sqrt,
                                               scale=1.0 / D, bias=EPS),
                 reads=[key + 'ss'], writes=[key + 'rs'])
            P.op('dve', lambda e: e.reciprocal(out=rs[:, 1:2], in_=rs[:, 0:1]),
                 reads=[key + 'rs'], writes=[key + 'rstd'])
            P.op('dve', lambda e: e.tensor_scalar(out=xt[:, :], in0=xt[:, :], scalar1=rs[:, 1:2],
                                                  scalar2=None, op0=ALU.mult),
                 reads=[key + 'xt', key + 'rstd'], writes=[key + 'xt'])
            for k4 in range(KC // 4 if 'tp' not in SKIP else 0):
                bank = pb[k4 % 2]
                bk = key + 'pst%d' % (k4 % 2)

                def tp(e, k4=k4, bank=bank):
                    for q in range(4):
                        kc = k4 * 4 + q
                        r = e.transpose(out=ps[bank][:, q * 128:(q + 1) * 128],
                                        in_=xt[:, kc * 128:(kc + 1) * 128], identity=ident[:, :])
                    return r
                P.op('pe', tp, reads=[key + 'xt', 'ident'], writes=[bk])
                for q in range(4 if 'evac' not in SKIP else 0):
                    kc = k4 * 4 + q
                    hk = key + 'hT%d' % kc
                    if hTf is not None:
                        P.op('act', lambda e, kc=kc, q=q, bank=bank: e.activation(
                            out=hTf[:, kc, :], in_=ps[bank][:, q * 128:(q + 1) * 128], func=AF.Identity,
                            scale=Aap[:, kc:kc + 1], bias=Bfn(kc)),
                            reads=[bk, 'A1', 'A2', 'modT'], writes=[hk + 'f'])
                        P.op('pool', lambda e, kc=kc: e.tensor_copy(out=hTdst(kc), in_=hTf[:, kc, :]),
                             reads=[hk + 'f'], writes=[hk])
                    elif k4 % 2 == 0:
                        P.op('dve', lambda e, kc=kc, q=q, bank=bank: e.tensor_scalar(
                            out=hTdst(kc), in0=ps[bank][:, q * 128:(q + 1) * 128],
                            scalar1=Aap[:, kc:kc + 1], scalar2=Bfn(kc), op0=ALU.mult, op1=ALU.add),
                            reads=[bk, 'A1', 'A2', 'modT'], writes=[hk])
                    else:
                        P.op('act', lambda e, kc=kc, q=q, bank=bank: e.activation(
                            out=hTdst(kc), in_=ps[bank][:, q * 128:(q + 1) * 128], func=AF.Identity,
                            scale=Aap[:, kc:kc + 1], bias=Bfn(kc)),
                            reads=[bk, 'A1', 'A2', 'modT'], writes=[hk])

        if stages >= 2:
            with ExitStack() as st:
                xts = [sb("xt%d" % i, [128, D], stack=st) for i in range(2)]
                junk = sb("junk", [128, D], BF16, stack=st)
                sss = [sb("ss%d" % i, [128, 1], stack=st) for i in range(2)]
                rss = [sb("rs%d" % i, [128, 2], stack=st) for i in range(2)]
                hTs = [sb("hTs%d" % i, [128, KC, 128], BF16, stack=st) for i in range(2)]
                for t in range(NWT):
                    b = t % 2
                    key = 's1_%d_' % b
                    norm_transpose(IN("xw")[t * 128:(t + 1) * 128, :], xts[b], junk, sss[b], rss[b],
                                   (lambda kc, b=b: hTs[b][:, kc, :]), A1, B1, key, (0, 1))
                    if 'store' in SKIP:
                        continue
                    P.dma('sp', hT_d[t, :, :], hTs[b][:, :, :].rearrange('p k t -> p (k t)'),
                          reads=[key + 'hT%d' % kc for kc in range(KC)], writes=['hT_d%d' % t])
                P.barrier()

        def W_IN(cb):
            return IN("w_in_t")[cb, :, :]

        cnt = {'l': 0, 'h': 0, 'b': 0, 'k': 0, 's': 0, 'y': 0}

        if stages >= 3:
            with ExitStack() as st:
                stg = [sb("wstg%d" % i, [128, KC * 128], stack=st) for i in range(2)]
                Wq = sb("Wq", [128, 2, KC, 128], BF16, stack=st)
                Wk = sb("Wk", [128, 2, KC, 128], BF16, stack=st)
                Wv = sb("Wv", [128, KC, 2, 128], BF16, stack=st)
                hTc = [sb("hTc%d" % i, [128, 2, KC, 128], BF16, stack=st) for i in range(2)]
                ssq_c = sb("ssq_c", [128, TOK], stack=st)
                ssq_a = sb("ssq_a", [128, TOK], stack=st)
                row1 = sb("row1", [1, TOK], stack=st)

                def load_block(cb, dst3, dkey):
                    i = cnt['l'] % 2
                    cnt['l'] += 1
                    P.dma('sp', stg[i][:, :], W_IN(cb), writes=['stg%d' % i])
                    src3 = stg[i][:, :].rearrange('p (k c) -> p k c', k=KC)
                    if i == 0:
                        P.op('pool', lambda e: e.tensor_copy(out=dst3, in_=src3), reads=['stg%d' % i], writes=[dkey])
                    else:
                        P.op('act', lambda e: e.copy(out=dst3, in_=src3), reads=['stg%d' % i], writes=[dkey])

                def load_hT(t0):
                    hb = cnt['h'] % 2
                    cnt['h'] += 1
                    P.dma('sp', hTc[hb][:, :, :, :].rearrange('p t k c -> p t (k c)'),
                          hT_d[t0:t0 + 2, :, :].rearrange('t p f -> p t f'), writes=['hTc%d' % hb])
                    return hb

                def proj_fm(wsrc, wkey, hb, bank, bkey):
                    def mm(e):
                        for kc in range(KC):
                            r = e.matmul(ps[bank][:, 0:256], lhsT=wsrc[:, kc, :], rhs=hTc[hb][:, :, kc, :],
                                         start=(kc == 0), stop=(kc == KC - 1))
                        return r
                    P.op('pe', mm, reads=[wkey, 'hTc%d' % hb], writes=[bkey])

                def finish_rstd(ssq, nfeat, dst, key):
                    P.op('dve', lambda e: e.tensor_scalar(out=row1[0:1, :], in0=ssq[0:1, :], scalar1=1.0 / nfeat,
                                                          scalar2=EPS, op0=ALU.mult, op1=ALU.add),
                         reads=[key], writes=['row1'])
                    P.op('act', lambda e: e.activation(out=row1[0:1, :], in_=row1[0:1, :], func=AF.Sqrt),
                         reads=['row1'], writes=['row1'])
                    P.op('dve', lambda e: e.reciprocal(out=row1[0:1, :], in_=row1[0:1, :]),
                         reads=['row1'], writes=['row1'])

                    def tr(e):
                        for t in range(8):
                            r = e.matmul(ps[2][:, t:t + 1], lhsT=row1[0:1, t * 128:(t + 1) * 128],
                                         rhs=ones_f[0:1, 0:1], start=True, stop=True)
                        return r
                    P.op('pe', tr, reads=['row1', 'ones_f'], writes=['pb2'])
                    P.op('dve', lambda e: e.tensor_copy(out=dst[:, :], in_=ps[2][:, 0:8]),
                         reads=['pb2'], writes=[key + 'rstd'])

                if LIM:
                    zt = sb('zt', [128, TOK], BF16, stack=st)
                    P.op('dve', lambda e: e.memset(zt[:, :], 0.0), writes=['zt'])
                    for k in range(KC):
                        P.dma('sp', yT_d[:, k, :], zt[:, :], reads=['zt'], writes=['yT_d%d' % k, 'yT_d%d_0' % k, 'yT_d%d_1' % k])
                with ExitStack() as s2:
                    u = sb("u", [128, 1280], stack=s2)
                    Bsb = sb("Bsb", [128, 1280], stack=s2)
                    Csb = sb("Csb", [128, 256], stack=s2)
                    tcv = sb("tcv", [128, TOK], stack=s2)
                    yc = sb("yc", [128, TOK], stack=s2)
                    ycsq = sb("ycsq", [128, TOK], BF16, stack=s2)
                    ycg = sb("ycg", [128, TOK], BF16, stack=s2)
                    cw = sb("cw", [128, 24], stack=s2)
                    cmask = sb("cmask_sb", [128, 2], stack=s2)
                    P.dma('sp', cw[:, :], IN("conv_w_t")[:, :], writes=['cw'])
                    P.dma('sp', cmask[:, :], IN("cmask")[:, :], writes=['cmask'])
                    P.op('dve', lambda e: e.memset(ssq_c[:, :], 0.0), writes=['ssq_c'])
                    for cblk in range(8 if not LIM else 1):
                        load_block(cblk, Wq[:, 0, :, :], 'Wq0')
                        load_block(8 + cblk, Wq[:, 1, :, :], 'Wq1')
                        load_block(16 + cblk, Wk[:, 0, :, :], 'Wk0')
                        for ch in range(5):
                            hb = load_hT(14 + 2 * ch)
                            c0 = ch * 256
                            proj_fm(Wq[:, 0, :, :], 'Wq0', hb, 0, 'pb0')
                            proj_fm(Wq[:, 1, :, :], 'Wq1', hb, 1, 'pb1')
                            proj_fm(Wk[:, 0, :, :], 'Wk0', hb, 3, 'pb3')
                            P.op('act', lambda e, c0=c0: e.copy(out=Bsb[:, c0:c0 + 256], in_=ps[0][:, 0:256]),
                                 reads=['pb0'], writes=['Bsb'])
                            P.op('act', lambda e: e.copy(out=Csb[:, :], in_=ps[1][:, 0:256]),
                                 reads=['pb1'], writes=['Csb'])
                            P.op('dve', lambda e, c0=c0: e.tensor_tensor(out=u[:, c0:c0 + 256], in0=ps[3][:, 0:256],
                                                                         in1=Csb[:, :], op=ALU.mult),
                                 reads=['pb3', 'Csb'], writes=['u'])
                        P.op('dve', lambda e: e.tensor_tensor(out=u[:, 254:256], in0=u[:, 254:256], in1=cmask[:, :],
                                                              op=ALU.mult), reads=['u', 'cmask'], writes=['u'])

                        def cwc(j, cblk=cblk):
                            return cw[:, cblk * 3 + j:cblk * 3 + j + 1]
                        P.op('dve', lambda e: e.tensor_scalar(out=tcv[:, :], in0=u[:, 256:1280], scalar1=cwc(2),
                                                              scalar2=None, op0=ALU.mult),
                             reads=['u', 'cw'], writes=['tcv'])
                        P.op('dve', lambda e: e.scalar_tensor_tensor(out=tcv[:, :], in0=u[:, 255:1279], scalar=cwc(1),
                                                                     in1=tcv[:, :], op0=ALU.mult, op1=ALU.add),
                             reads=['u', 'cw', 'tcv'], writes=['tcv'])
                        P.op('dve', lambda e: e.scalar_tensor_tensor(out=tcv[:, :], in0=u[:, 254:1278], scalar=cwc(0),
                                                                     in1=tcv[:, :], op0=ALU.mult, op1=ALU.add),
                             reads=['u', 'cw', 'tcv'], writes=['tcv'])
                        P.op('dve', lambda e: e.tensor_tensor(out=yc[:, :], in0=Bsb[:, 256:1280], in1=tcv[:, :],
                                                              op=ALU.mult), reads=['Bsb', 'tcv'], writes=['yc'])
                        P.op('pool', lambda e: e.tensor_tensor(out=ycsq[:, :], in0=yc[:, :], in1=yc[:, :], op=ALU.mult),
                             reads=['yc'], writes=['ycsq'])
                        P.op('pool', lambda e, cblk=cblk: e.tensor_scalar(out=ycg[:, :], in0=yc[:, :],
                                                                          scalar1=gbr[:, cblk:cblk + 1], scalar2=None,
                                                                          op0=ALU.mult),
                             reads=['yc', 'gbr'], writes=['ycg'])
                        P.dma('sp', yT_d[:, cblk, :], ycg[:, :], reads=['ycg'], writes=['yT_d%d' % cblk])
                        for n in range(2):
                            P.op('pe', lambda e, n=n: e.matmul(ps[2][:, :], lhsT=ones_b[:, :],
                                                               rhs=ycsq[:, n * 512:(n + 1) * 512], start=True, stop=True),
                                 reads=['ycsq', 'ones_b'], writes=['pb2'])
                            P.op('dve', lambda e, n=n: e.tensor_tensor(out=ssq_c[:, n * 512:(n + 1) * 512],
                                                                       in0=ps[2][:, :], in1=ssq_c[:, n * 512:(n + 1) * 512],
                                                                       op=ALU.add),
                                 reads=['pb2', 'ssq_c'], writes=['ssq_c'])
                    finish_rstd(ssq_c, 1024.0, rstd_c, 'ssq_c')
                    P.barrier()

                if stages >= 4:
                    with ExitStack() as s3:
                        KT = sb("KT", [128, 2, WIN], BF16, stack=s3)
                        QT = sb("QT", [128, 2, TOK], BF16, stack=s3)
                        Vsb = sb("Vsb", [128, NWT, 256], BF16, stack=s3)
                        tdel = sb("tdel", [128, SLEN], stack=s3)
                        tmul = sb("tmul", [128, SLEN], BF16, stack=s3)
                        kbias = sb("kbias", [128, NWT], stack=s3)
                        kfb = [sb("kfb%d" % i, [128, 256], stack=s3) for i in range(2)]
                        sqb = [sb("sqb%d" % i, [128, 256], BF16, stack=s3) for i in range(2)]
                        v1 = sb("v1", [128, 256], stack=s3)
                        v2 = sb("v2", [128, 256], stack=s3)
                        v3 = sb("v3", [128, 256], stack=s3)
                        ssb = [sb("ssb%d" % i, [128, 512], stack=s3) for i in range(4)]
                        eb = [sb("eb%d" % i, [128, 512], BF16, stack=s3) for i in range(4)]
                        pTb = [sb("pTb%d" % i, [128, 512], BF16, stack=s3) for i in range(4)]
                        rec = sb("rec", [128, 512], stack=s3)
                        yaf = sb("yaf", [128, 512], stack=s3)
                        ysq = sb("ysq", [128, 512], BF16, stack=s3)
                        yst = [sb("yst%d" % i, [128, 512], BF16, stack=s3) for i in range(2)]
                        qgs = sb("qgs", [128, 1], stack=s3)
                        kgs = sb("kgs", [128, 1], stack=s3)
                        P.dma('sp', tdel[:, :], IN("tdelta")[:, :], writes=['tdel'])
                        P.dma('sp', tmul[:, :], IN("tmult")[:, :], writes=['tmul'])
                        P.dma('sp', kbias[:, :], IN("keybias")[:, :], writes=['kbias'])
                        P.dma('sp', qgs[:, :], IN("qg")[:, :], writes=['qgs'])
                        P.dma('sp', kgs[:, :], IN("kg")[:, :], writes=['kgs'])
                        P.op('dve', lambda e: e.tensor_scalar(out=qgs[:, :], in0=qgs[:, :], scalar1=float(128 ** -0.5),
                                                              scalar2=None, op0=ALU.mult), reads=['qgs'], writes=['qgs'])
                        P.op('dve', lambda e: e.memset(ssq_a[:, :], 0.0), writes=['ssq_a'])
                        SL = slopes()

                        def qknormA(bank):
                            i = cnt['k'] % 2
                            cnt['k'] += 1
                            bkey = 'pb%d' % bank
                            P.op('act', lambda e: e.copy(out=kfb[i][:, :], in_=ps[bank][:, 0:256]),
                                 reads=[bkey], writes=['kf%d' % i])
                            P.op('pool', lambda e: e.tensor_tensor(out=sqb[i][:, :], in0=kfb[i][:, :], in1=kfb[i][:, :],
                                                                   op=ALU.mult), reads=['kf%d' % i], writes=['sqb%d' % i])
                            return i

                        def qknormB(i, dst_ap, gsc, gkey, dkey):
                            P.op('pe', lambda e: e.matmul(ps[2][:, 0:256], lhsT=ones_b[:, :], rhs=sqb[i][:, :],
                                                          start=True, stop=True),
                                 reads=['sqb%d' % i, 'ones_b'], writes=['pb2'])
                            P.op('dve', lambda e: e.tensor_scalar(out=v1[:, :], in0=ps[2][:, 0:256], scalar1=1.0 / 128,
                                                                  scalar2=EPS, op0=ALU.mult, op1=ALU.add),
                                 reads=['pb2'], writes=['v1'])
                            P.op('act', lambda e: e.activation(out=v2[:, :], in_=v1[:, :], func=AF.Sqrt),
                                 reads=['v1'], writes=['v2'])
                            P.op('dve', lambda e: e.reciprocal(out=v3[:, :], in_=v2[:, :]), reads=['v2'], writes=['v3'])
                            P.op('dve', lambda e: e.scalar_tensor_tensor(out=dst_ap, in0=kfb[i][:, :], scalar=gsc[:, 0:1],
                                                                         in1=v3[:, :], op0=ALU.mult, op1=ALU.mult),
                                 reads=['kf%d' % i, 'v3', gkey], writes=[dkey])

                        def load_pair_weights(hp):
                            h0 = 2 * hp
                            load_block(24 + h0, Wq[:, 0, :, :], 'Wq0')
                            load_block(24 + h0 + 1, Wq[:, 1, :, :], 'Wq1')
                            load_block(48 + h0, Wk[:, 0, :, :], 'Wk0')
                            load_block(48 + h0 + 1, Wk[:, 1, :, :], 'Wk1')
                            load_block(72 + h0, Wv[:, :, 0, :], 'Wv0')
                            load_block(72 + h0 + 1, Wv[:, :, 1, :], 'Wv1')

                        NHP = 12 if not LIM else 1
                        load_pair_weights(0)
                        for hp in range(NHP):
                            h0 = 2 * hp
                            for ch in range(12):
                                hb = load_hT(2 * ch)
                                ks = []
                                for hh in range(2):
                                    proj_fm(Wk[:, hh, :, :], 'Wk%d' % hh, hb, hh, 'pb%d' % hh)
                                    ks.append(qknormA(hh))
                                for t in range(2):
                                    def mmv(e, t=t, hb=hb):
                                        for kc in range(KC):
                                            r = e.matmul(ps[3][:, 0:256], lhsT=hTc[hb][:, t, kc, :], rhs=Wv[:, kc, :, :],
                                                         start=(kc == 0), stop=(kc == KC - 1))
                                        return r
                                    P.op('pe', mmv, reads=['Wv0', 'Wv1', 'hTc%d' % hb], writes=['pb3'])
                                    P.op('dve', lambda e, t=t, ch=ch: e.tensor_copy(out=Vsb[:, 2 * ch + t, :],
                                                                                    in_=ps[3][:, 0:256]),
                                         reads=['pb3'], writes=['V%d' % (2 * ch + t)])
                                for hh in range(2):
                                    qknormB(ks[hh], KT[:, hh, ch * 256:(ch + 1) * 256], kgs, 'kgs', 'KT%d_%d' % (hh, ch))
                                if ch >= 8:
                                    qs = []
                                    for hh in range(2):
                                        proj_fm(Wq[:, hh, :, :], 'Wq%d' % hh, hb, hh, 'pb%d' % hh)
                                        qs.append(qknormA(hh))
                                    for hh in range(2):
                                        qknormB(qs[hh], QT[:, hh, (ch - 8) * 256:(ch - 7) * 256], qgs, 'qgs',
                                                'QT%d_%d' % (hh, ch - 8))
                            if hp + 1 < NHP:
                                load_pair_weights(hp + 1)
                            for hh in range(2):
                                h = h0 + hh
                                for g in range(2):
                                    qb0 = 16 + 4 * g
                                    kaps = list(range(4 * g, 4 * g + 20))

                                    SB = (4, 5, 0, 1)
                                    SK = ('pS0', 'pS1', 'pb0', 'pb1')

                                    def issue_S(kap, sbk, hh=hh, g=g):
                                        P.op('pe', lambda e: e.matmul(ps[SB[sbk]][:, :], lhsT=KT[:, hh, kap * 128:(kap + 1) * 128],
                                                                      rhs=QT[:, hh, g * 512:(g + 1) * 512], start=True, stop=True),
                                             reads=['KT%d_%d' % (hh, kap // 2), 'QT%d_%d' % (hh, 2 * g), 'QT%d_%d' % (hh, 2 * g + 1)],
                                             writes=[SK[sbk]])
                                    sb0 = cnt['s']
                                    LA = 3
                                    for a in range(LA):
                                        issue_S(kaps[a], (sb0 + a) % 4)
                                    for idx, kap in enumerate(kaps):
                                        sbk = (sb0 + idx) % 4
                                        if idx + LA < len(kaps):
                                            issue_S(kaps[idx + LA], (sb0 + idx + LA) % 4)
                                        off = 128 * (qb0 - kap) + SOFF
                                        P.op('dve', lambda e, off=off, sbk=sbk, h=h: e.scalar_tensor_tensor(
                                            out=ssb[sbk][:, :], in0=tdel[:, off:off + 512], scalar=SL[h],
                                            in1=ps[SB[sbk]][:, :], op0=ALU.mult, op1=ALU.add),
                                            reads=[SK[sbk], 'tdel'], writes=['ssb%d' % sbk])
                                        P.op('act', lambda e, sbk=sbk, kap=kap: e.activation(
                                            out=eb[sbk][:, :], in_=ssb[sbk][:, :], func=AF.Exp, bias=kbias[:, kap:kap + 1]),
                                            reads=['ssb%d' % sbk, 'kbias'], writes=['eb%d' % sbk])
                                        P.op('pool', lambda e, sbk=sbk, off=off: e.tensor_tensor(
                                            out=pTb[sbk][:, :], in0=eb[sbk][:, :], in1=tmul[:, off:off + 512], op=ALU.mult),
                                            reads=['eb%d' % sbk, 'tmul'], writes=['pT%d' % sbk])

                                        def pv(e, sbk=sbk, kap=kap, idx=idx, hh=hh):
                                            e.matmul(ps[6][:, :], lhsT=Vsb[:, kap, hh * 128:(hh + 1) * 128], rhs=pTb[sbk][:, :],
                                                     start=(idx == 0), stop=(idx == 19))
                                            return e.matmul(ps[7][:, :], lhsT=ones_b[:, :], rhs=pTb[sbk][:, :],
                                                            start=(idx == 0), stop=(idx == 19))
                                        P.op('pe', pv, reads=['pT%d' % sbk, 'V%d' % kap, 'ones_b'], writes=['pO'])
                                    cnt['s'] = sb0 + len(kaps)
                                    P.op('dve', lambda e: e.reciprocal(out=rec[:, :], in_=ps[7][:, :]),
                                         reads=['pO'], writes=['rec'])
                                    P.op('dve', lambda e: e.tensor_tensor(out=yaf[:, :], in0=ps[6][:, :], in1=rec[:, :],
                                                                          op=ALU.mult), reads=['pO', 'rec'], writes=['yaf'])
                                    P.op('pool', lambda e: e.tensor_tensor(out=ysq[:, :], in0=yaf[:, :], in1=yaf[:, :],
                                                                           op=ALU.mult), reads=['yaf'], writes=['ysq'])
                                    P.op('pe', lambda e: e.matmul(ps[2][:, :], lhsT=ones_b[:, :], rhs=ysq[:, :],
                                                                  start=True, stop=True),
                                         reads=['ysq', 'ones_b'], writes=['pb2'])
                                    P.op('dve', lambda e, g=g: e.tensor_tensor(out=ssq_a[:, g * 512:(g + 1) * 512],
                                                                               in0=ps[2][:, :], in1=ssq_a[:, g * 512:(g + 1) * 512],
                                                                               op=ALU.add),
                                         reads=['pb2', 'ssq_a'], writes=['ssq_a'])
                                    yi = cnt['y'] % 2
                                    cnt['y'] += 1
                                    P.op('pool', lambda e, yi=yi, h=h: e.tensor_scalar(out=yst[yi][:, :], in0=yaf[:, :],
                                                                                       scalar1=gbr[:, 8 + h:9 + h], scalar2=None,
                                                                                       op0=ALU.mult),
                                         reads=['yaf', 'gbr'], writes=['yst%d' % yi])
                                    P.dma('sp', yT_d[:, 8 + h, g * 512:(g + 1) * 512], yst[yi][:, :],
                                          reads=['yst%d' % yi], writes=['yT_d%d_%d' % (8 + h, g)])
                        finish_rstd(ssq_a, 3072.0, rstd_a, 'ssq_a')
                        P.barrier()
                if dbg:
                    P.dma('sp', dbg_t[:, 0:8], rstd_c[:, :], reads=['ssq_crstd'], writes=['dbg'])
                    if stages >= 4:
                        P.dma('sp', dbg_t[:, 8:16], rstd_a[:, :], reads=['ssq_arstd'], writes=['dbg'])
                P.barrier()

        if stages >= 5:
            with ExitStack() as st:
                yT = sb("yT", [128, KC, TOK], BF16, stack=st)
                gt1b = sb("gt1b", [128, D], stack=st)
                wostg = [sb("wostg%d" % i, [128, 8, 512], stack=st) for i in range(2)]
                Wo2 = [sb("Wo%d" % i, [128, KC, 512], BF16, stack=st) for i in range(2)]
                xs = [sb("xs%d" % i, [128, 512], stack=st) for i in range(2)]
                t1 = [sb("t1_%d" % i, [128, 512], stack=st) for i in range(2)]
                x2s = [sb("x2s%d" % i, [128, 512], stack=st) for i in range(2)]
                for k8 in range(4):
                    P.dma('sp', yT[:, k8 * 8:(k8 + 1) * 8, :], yT_d[:, k8 * 8:(k8 + 1) * 8, :], writes=['yT%d' % k8])
                P.dma('sp', gt1b[:, :], gtb_d[0, :, :], writes=['gt1b'])
                nw = 0
                nt = 0
                def load_wo(cb):
                    nonlocal_nw = cnt.setdefault('wo', 0)
                    for k8 in range(4):
                        i = cnt['wo'] % 2
                        cnt['wo'] += 1
                        P.dma('sp', wostg[i][:, :, :],
                              IN("w_out")[k8 * 1024:(k8 + 1) * 1024, cb * 512:(cb + 1) * 512].rearrange('(k p) c -> p k c', p=128),
                              writes=['wostg%d' % i])
                        dst = Wo2[cb % 2][:, k8 * 8:(k8 + 1) * 8, :]
                        if i == 0:
                            P.op('pool', lambda e, dst=dst, i=i: e.tensor_copy(out=dst, in_=wostg[i][:, :, :]),
                                 reads=['wostg%d' % i], writes=['Wo%d_%d' % (cb % 2, k8)])
                        else:
                            P.op('act', lambda e, dst=dst, i=i: e.copy(out=dst, in_=wostg[i][:, :, :]),
                                 reads=['wostg%d' % i], writes=['Wo%d_%d' % (cb % 2, k8)])
                load_wo(0)
                for cb in range(8):
                    if cb + 1 < 8:
                        load_wo(cb + 1)
                    Wo = Wo2[cb % 2]
                    wkeys = ['Wo%d_%d' % (cb % 2, k8) for k8 in range(4)]
                    for tile in range(8):
                        j = nt % 2
                        nt += 1
                        bc, ba = (0, 1) if j == 0 else (2, 3)

                        def mmo(e, tile=tile, bc=bc, ba=ba, Wo=Wo):
                            for kc in range(8):
                                e.matmul(ps[bc][:, :], lhsT=yT[:, kc, tile * 128:(tile + 1) * 128], rhs=Wo[:, kc, :],
                                         start=(kc == 0), stop=(kc == 7))
                            for kc in range(8, KC):
                                r = e.matmul(ps[ba][:, :], lhsT=yT[:, kc, tile * 128:(tile + 1) * 128], rhs=Wo[:, kc, :],
                                             start=(kc == 8), stop=(kc == KC - 1))
                            return r
                        P.op('pe', mmo, reads=['yT0', 'yT1', 'yT2', 'yT3'] + wkeys, writes=['po%d' % j])
                        P.dma('sp', xs[j][:, :], IN("xw")[HALO + tile * 128:HALO + (tile + 1) * 128, cb * 512:(cb + 1) * 512],
                              writes=['xs%d' % j])
                        P.op('dve', lambda e, j=j, tile=tile, bc=bc: e.tensor_scalar(
                            out=t1[j][:, :], in0=ps[bc][:, :], scalar1=rstd_c[:, tile:tile + 1], scalar2=None, op0=ALU.mult),
                            reads=['po%d' % j], writes=['t1_%d' % j])
                        P.op('dve', lambda e, j=j, tile=tile, ba=ba: e.scalar_tensor_tensor(
                            out=t1[j][:, :], in0=ps[ba][:, :], scalar=rstd_a[:, tile:tile + 1], in1=t1[j][:, :],
                            op0=ALU.mult, op1=ALU.add), reads=['po%d' % j, 't1_%d' % j], writes=['t1_%d' % j])
                        P.op('dve', lambda e, j=j, cb=cb: e.tensor_tensor(
                            out=t1[j][:, :], in0=t1[j][:, :], in1=gt1b[:, cb * 512:(cb + 1) * 512], op=ALU.mult),
                            reads=['t1_%d' % j, 'gt1b'], writes=['t1_%d' % j])
                        P.op('dve', lambda e, j=j: e.tensor_tensor(out=x2s[j][:, :], in0=t1[j][:, :], in1=xs[j][:, :], op=ALU.add),
                             reads=['t1_%d' % j, 'xs%d' % j], writes=['x2s%d' % j])
                        P.dma('sp', out[tile * 128:(tile + 1) * 128, cb * 512:(cb + 1) * 512], x2s[j][:, :],
                              reads=['x2s%d' % j], writes=['out_%d_%d' % (tile, cb)])
                P.barrier()
            P.barrier()

        if stages >= 6:
            with ExitStack() as st:
                h2T = sb("h2T", [128, KC, TOK], BF16, stack=st)
                with ExitStack() as s5:
                    xt5 = sb("xt5", [128, D], stack=s5)
                    junk5 = sb("junk5", [128, D], BF16, stack=s5)
                    ss5 = sb("ss5", [128, 1], stack=s5)
                    rs5 = sb("rs5", [128, 2], stack=s5)
                    hTf = sb("hTf", [128, KC, 128], stack=s5)
                    wr = sb("wr", [128, KC, 72], stack=s5)
                    L = sb("L", [128, 72], stack=s5)
                    sm = sb("sm", [128, 16], stack=s5)
                    goh = sb("goh", [128, 8], stack=s5)
                    gex = sb("gex", [128, 8], stack=s5)
                    ein = sb("ein", [128, 8], stack=s5)
                    e2 = sb("e2", [128, 8], stack=s5)
                    oh1 = sb("oh1", [128, 8], stack=s5)
                    oh2 = sb("oh2", [128, 8], stack=s5)
                    wsel = sb("wsel", [128, 8], stack=s5)
                    wgt = sb("wgt", [128, 64], stack=s5)
                    wgtTs = sb("wgtTs", [64, 128], stack=s5)
                    P.dma('sp', wr[:, :, :].rearrange('p k c -> p (k c)'), IN("w_r")[:, :], writes=['wr'])
                    for tile in range(8):
                        key = 's5_'
                        norm_transpose(out[tile * 128:(tile + 1) * 128, :], xt5, junk5, ss5, rs5,
                                       (lambda kc, tile=tile: h2T[:, kc, tile * 128:(tile + 1) * 128]),
                                       A2, B2, key, (0, 1), hTf=hTf)

                        def mml(e):
                            for kc in range(KC):
                                r = e.matmul(ps[2][:, 0:72], lhsT=hTf[:, kc, :], rhs=wr[:, kc, :],
                                             start=(kc == 0), stop=(kc == KC - 1))
                            return r
                        P.op('pe', mml, reads=['wr'] + [key + 'hT%df' % kc for kc in range(KC)], writes=['pb2'])
                        V = lambda e: e
                        P.op('dve', lambda e: e.tensor_copy(out=L[:, :], in_=ps[2][:, 0:72]), reads=['pb2'], writes=['R'])
                        P.op('dve', lambda e: e.tensor_reduce(out=sm[:, 0:1], in_=L[:, 0:8], axis=AX.X, op=ALU.max),
                             reads=['R'], writes=['R'])
                        P.op('dve', lambda e: e.tensor_scalar(out=goh[:, :], in0=L[:, 0:8], scalar1=sm[:, 0:1], scalar2=None,
                                                              op0=ALU.is_equal), reads=['R'], writes=['R'])
                        P.op('dve', lambda e: e.tensor_scalar(out=sm[:, 1:2], in0=sm[:, 0:1], scalar1=-1.0, scalar2=None,
                                                              op0=ALU.mult), reads=['R'], writes=['R'])
                        P.op('dve', lambda e: e.memset(sm[:, 2:3], 0.0), reads=['R'], writes=['R'])
                        P.op('act', lambda e: e.activation(out=gex[:, :], in_=L[:, 0:8], func=AF.Exp, bias=sm[:, 1:2],
                                                           accum_out=sm[:, 2:3]), reads=['R'], writes=['R'])
                        P.op('dve', lambda e: e.reciprocal(out=sm[:, 3:4], in_=sm[:, 2:3]), reads=['R'], writes=['R'])
                        P.op('dve', lambda e: e.tensor_scalar(out=ein[:, :], in0=L[:, 8:16], scalar1=goh[:, 0:1], scalar2=None,
                                                              op0=ALU.mult), reads=['R'], writes=['R'])
                        for g in range(1, 8):
                            P.op('dve', lambda e, g=g: e.scalar_tensor_tensor(
                                out=ein[:, :], in0=L[:, 8 + 8 * g:16 + 8 * g], scalar=goh[:, g:g + 1], in1=ein[:, :],
                                op0=ALU.mult, op1=ALU.add), reads=['R'], writes=['R'])
                        P.op('dve', lambda e: e.tensor_reduce(out=sm[:, 4:5], in_=ein[:, :], axis=AX.X, op=ALU.max),
                             reads=['R'], writes=['R'])
                        P.op('dve', lambda e: e.tensor_scalar(out=oh1[:, :], in0=ein[:, :], scalar1=sm[:, 4:5], scalar2=None,
                                                              op0=ALU.is_equal), reads=['R'], writes=['R'])
                        P.op('dve', lambda e: e.scalar_tensor_tensor(out=e2[:, :], in0=oh1[:, :], scalar=-1.0e30, in1=ein[:, :],
                                                                     op0=ALU.mult, op1=ALU.add), reads=['R'], writes=['R'])
                        P.op('dve', lambda e: e.tensor_reduce(out=sm[:, 5:6], in_=e2[:, :], axis=AX.X, op=ALU.max),
                             reads=['R'], writes=['R'])
                        P.op('dve', lambda e: e.tensor_scalar(out=oh2[:, :], in0=e2[:, :], scalar1=sm[:, 5:6], scalar2=None,
                                                              op0=ALU.is_equal), reads=['R'], writes=['R'])
                        P.op('dve', lambda e: e.tensor_tensor(out=sm[:, 6:7], in0=sm[:, 5:6], in1=sm[:, 4:5], op=ALU.subtract),
                             reads=['R'], writes=['R'])
                        P.op('act', lambda e: e.activation(out=sm[:, 7:8], in_=sm[:, 6:7], func=AF.Exp), reads=['R'], writes=['R'])
                        P.op('dve', lambda e: e.tensor_scalar(out=sm[:, 8:9], in0=sm[:, 7:8], scalar1=1.0, scalar2=None,
                                                              op0=ALU.add), reads=['R'], writes=['R'])
                        P.op('dve', lambda e: e.reciprocal(out=sm[:, 9:10], in_=sm[:, 8:9]), reads=['R'], writes=['R'])
                        P.op('dve', lambda e: e.tensor_tensor(out=sm[:, 10:11], in0=sm[:, 7:8], in1=sm[:, 9:10], op=ALU.mult),
                             reads=['R'], writes=['R'])
                        P.op('dve', lambda e: e.tensor_scalar(out=sm[:, 9:11], in0=sm[:, 9:11], scalar1=sm[:, 3:4], scalar2=None,
                                                              op0=ALU.mult), reads=['R'], writes=['R'])
                        P.op('dve', lambda e: e.tensor_scalar(out=wsel[:, :], in0=oh1[:, :], scalar1=sm[:, 9:10], scalar2=None,
                                                              op0=ALU.mult), reads=['R'], writes=['R'])
                        P.op('dve', lambda e: e.scalar_tensor_tensor(out=wsel[:, :], in0=oh2[:, :], scalar=sm[:, 10:11],
                                                                     in1=wsel[:, :], op0=ALU.mult, op1=ALU.add),
                             reads=['R'], writes=['R'])
                        for g in range(8):
                            P.op('dve', lambda e, g=g: e.tensor_scalar(out=wgt[:, 8 * g:8 * g + 8], in0=wsel[:, :],
                                                                       scalar1=goh[:, g:g + 1], scalar2=None, op0=ALU.mult),
                                 reads=['R', 'wgtrd'], writes=['R'])
                        P.op('pe', lambda e: e.transpose(out=ps[3][0:64, 0:128], in_=wgt[:, 0:64], identity=ident[:, :]),
                             reads=['R', 'ident'], writes=['pb3', 'wgtrd'])
                        P.op('dve', lambda e: e.tensor_copy(out=wgtTs[:, :], in_=ps[3][0:64, 0:128]),
                             reads=['pb3'], writes=['wgtTs'])
                        P.dma('sp', wgtT_d[:, tile * 128:(tile + 1) * 128], wgtTs[:, :], reads=['wgtTs'], writes=['wgtT_d'])
                    P.barrier()
                if stages >= 7:
                    with ExitStack() as s6:
                        wstg6 = [sb("wstg6_%d" % i, [128, (KC // 2) * 128], stack=s6) for i in range(4)]
                        W13 = [[sb("W13_%d_%d" % (a, i), [128, KC, 128], BF16, stack=s6) for i in range(2)] for a in range(2)]
                        actT = sb("actT", [128, 2, 4, TOK], BF16, stack=s6)
                        wb = [sb("wb%d" % i, [128, TOK], stack=s6) for i in range(2)]
                        s1 = [sb("s1_%d" % i, [128, 512], stack=s6) for i in range(2)]
                        w2stg = [sb("w2stg%d" % i, [128, 4 * 512], stack=s6) for i in range(2)]
                        W2b = [sb("W2b%d" % i, [128, 2, 4, 512], BF16, stack=s6) for i in range(2)]
                        gt2s = [sb("gt2s%d" % i, [128, 512], stack=s6) for i in range(2)]
                        NOST = 6
                        ost = [sb("ost%d" % i, [128, 512], stack=s6) for i in range(NOST)]
                        NP = NEXP // 2 if not LIM else 1
                        steps = []
                        na = 0
                        nb = 0
                        for pair in range(NP):
                            for el in range(2):
                                for j in range(4):
                                    steps.append(('A', pair, el, j, na % 2))
                                    na += 1
                            for cb in range(8):
                                steps.append(('B', pair, cb, nb % 2))
                                nb += 1
                        cn = {'l': 0, 'w2': 0, 'o': 0}

                        def cast3(i, dst, src3, skey, dkey, engs):
                            eng = engs[i]
                            if eng == 'act':
                                P.op('act', lambda e: e.copy(out=dst, in_=src3), reads=[skey], writes=[dkey])
                            else:
                                P.op(eng, lambda e: e.tensor_copy(out=dst, in_=src3), reads=[skey], writes=[dkey])

                        def prefetch(stp):
                            if stp[0] == 'A':
                                _, pair, el, j, a = stp
                                ex = pair * 2 + el
                                if j == 0:
                                    P.dma('sp', wb[el][:, :], wgtT_d[ex:ex + 1, :].to_broadcast([128, TOK]), writes=['wb%d' % el])
                                for wi, wname in enumerate(("w1_t", "w3_t")):
                                    for hf in range(2):
                                        i = cn['l'] % 4
                                        cn['l'] += 1
                                        P.dma('sp', wstg6[i][:, :], IN(wname)[ex, j, :, hf * 2048:(hf + 1) * 2048], writes=['wstg6_%d' % i])
                                        src3 = wstg6[i][:, :].rearrange('p (k c) -> p k c', k=KC // 2)
                                        cast3((wi + hf) % 2, W13[a][wi][:, hf * 16:(hf + 1) * 16, :], src3, 'wstg6_%d' % i,
                                              'W13_%d_%d_%d' % (a, wi, hf), ('dve', 'act'))
                            else:
                                _, pair, cb, wa_ = stp
                                for el in range(2):
                                    ex = pair * 2 + el
                                    i = cn['w2'] % 2
                                    cn['w2'] += 1
                                    P.dma('sp', w2stg[i][:, :], IN("w2_t")[ex, cb, :, :], writes=['w2stg%d' % i])
                                    src3 = w2stg[i][:, :].rearrange('p (k c) -> p k c', k=4)
                                    cast3(i, W2b[wa_][:, el, :, :], src3, 'w2stg%d' % i, 'W2b%d_%d' % (wa_, el), ('dve', 'act'))
                                P.dma('sp', gt2s[wa_][:, :], gtb_d[1, :, cb * 512:(cb + 1) * 512], writes=['gt2s%d' % wa_])

                        def compute(stp):
                            if stp[0] == 'A':
                                _, pair, el, j, a = stp

                                for chk in range(2):
                                    def mm13(e, chk=chk):
                                        for wi in range(2):
                                            for kc in range(KC):
                                                r = e.matmul(ps[2 * chk + wi][:, :], lhsT=W13[a][wi][:, kc, :],
                                                             rhs=h2T[:, kc, chk * 512:(chk + 1) * 512],
                                                             start=(kc == 0), stop=(kc == KC - 1))
                                        return r
                                    P.op('pe', mm13, reads=['W13_%d_%d_%d' % (a, wi_, hf_) for wi_ in range(2) for hf_ in range(2)] + ['h2T'], writes=['pa%d' % chk])
                                for chk in range(2):
                                    P.op('act', lambda e, chk=chk: e.activation(out=s1[chk][:, :], in_=ps[2 * chk][:, :], func=AF.Silu),
                                         reads=['pa%d' % chk], writes=['s1_%d' % chk])
                                    P.op('dve', lambda e, chk=chk: e.tensor_tensor(out=s1[chk][:, :], in0=ps[2 * chk + 1][:, :],
                                                                                  in1=s1[chk][:, :], op=ALU.mult),
                                         reads=['pa%d' % chk, 's1_%d' % chk], writes=['s1_%d' % chk])
                                    P.op('pool', lambda e, chk=chk: e.tensor_tensor(
                                        out=actT[:, el, j, chk * 512:(chk + 1) * 512], in0=s1[chk][:, :],
                                        in1=wb[el][:, chk * 512:(chk + 1) * 512], op=ALU.mult),
                                        reads=['s1_%d' % chk, 'wb%d' % el], writes=['actT'])
                            else:
                                _, pair, cb, wa_ = stp
                                for tile in range(8):
                                    o = cn['o'] % 2
                                    ob = cn['o'] % NOST
                                    cn['o'] += 1

                                    def mm2(e, tile=tile, o=o):
                                        n = 0
                                        for el in range(2):
                                            for kc in range(4):
                                                r = e.matmul(ps[4 + o][:, :], lhsT=actT[:, el, kc, tile * 128:(tile + 1) * 128],
                                                             rhs=W2b[wa_][:, el, kc, :], start=(n == 0), stop=(n == 7))
                                                n += 1
                                        return r
                                    P.op('pe', mm2, reads=['actT', 'W2b%d_0' % wa_, 'W2b%d_1' % wa_], writes=['pm%d' % o])
                                    P.op('dve', lambda e, o=o, ob=ob: e.tensor_tensor(out=ost[ob][:, :], in0=ps[4 + o][:, :],
                                                                                     in1=gt2s[wa_][:, :], op=ALU.mult),
                                         reads=['pm%d' % o, 'gt2s%d' % wa_], writes=['ost%d' % ob])
                                    P.dma('pool', out[tile * 128:(tile + 1) * 128, cb * 512:(cb + 1) * 512], ost[ob][:, :],
                                          reads=['ost%d' % ob], writes=['out_%d_%d' % (tile, cb)], accum_op=ALU.add)

                        prefetch(steps[0])
                        for si, stp in enumerate(steps):
                            if si + 1 < len(steps):
                                prefetch(steps[si + 1])
                            compute(stp)
                        P.barrier()
            P.barrier()

        P.barrier()
    return nc, sorted(_in)


def _mult(delta):
    d = np.asarray(delta)
    m = ((d >= 0) & (d <= 128)).astype(np.float32)
    m += ((d >= 0) & (d <= 512) & (d % 4 == 0)).astype(np.float32)
    m += ((d >= 0) & (d <= 2048) & (d % 16 == 0)).astype(np.float32)
    return m


def host_consts():
    import ml_dtypes
    u = np.arange(128)[:, None]
    cidx = np.arange(SLEN)[None, :]
    delta = cidx - SOFF - u
    tdelta = (-np.maximum(delta, 0)).astype(np.float32)
    tmult = _mult(delta).astype(ml_dtypes.bfloat16)
    return {"ident": np.eye(128, dtype=np.float32), "tdelta": np.ascontiguousarray(tdelta),
            "tmult": np.ascontiguousarray(tmult)}


def t32(v):
    return np.ascontiguousarray(np.asarray(v, np.float32).reshape(KC, 128).T)


def host_shared(inp, need):
    sh = {}
    if "c_t" in need:
        sh["c_t"] = t32(inp["c"][0])
    if "w_ada" in need:
        sh["w_ada"] = np.ascontiguousarray(inp["w_ada"][0])
    if "b_ada" in need:
        sh["b_ada"] = np.ascontiguousarray(inp["b_ada"][0][None, :])
    if "g_mix_t" in need:
        sh["g_mix_t"] = t32(inp["g_mix"][0])
    if "g_ffn_t" in need:
        sh["g_ffn_t"] = t32(inp["g_ffn"][0])
    if "g_br_t" in need:
        sh["g_br_t"] = t32(inp["g_branch"][0])
    if "w_in_t" in need:
        w = inp["w_in"][0].reshape(KC, 128, 96, 128).transpose(2, 1, 0, 3)
        sh["w_in_t"] = np.ascontiguousarray(w).reshape(96, 128, KC * 128)
    if "conv_w_t" in need:
        cw = inp["conv_w"][0].reshape(3, 8, 128).transpose(2, 1, 0)
        sh["conv_w_t"] = np.ascontiguousarray(cw).reshape(128, 24)
    if "qg" in need:
        sh["qg"] = np.ascontiguousarray(inp["q_norm_g"][0][:, None])
    if "kg" in need:
        sh["kg"] = np.ascontiguousarray(inp["k_norm_g"][0][:, None])
    if "w_out" in need:
        sh["w_out"] = np.ascontiguousarray(inp["w_out"][0])
    if "w_r" in need:
        wr = np.concatenate([inp["w_group"][0], inp["w_expert"][0]], axis=1)
        sh["w_r"] = np.ascontiguousarray(wr.reshape(KC, 128, 72).transpose(1, 0, 2)).reshape(128, KC * 72)
    for nm, src in (("w1_t", "w1"), ("w3_t", "w3")):
        if nm in need:
            w = inp[src][0].reshape(NEXP, KC, 128, 4, 128).transpose(0, 3, 2, 1, 4)
            sh[nm] = np.ascontiguousarray(w).reshape(NEXP, 4, 128, KC * 128)
    if "w2_t" in need:
        w = inp["w2"][0].reshape(NEXP, 4, 128, 8, 512).transpose(0, 3, 2, 1, 4)
        sh["w2_t"] = np.ascontiguousarray(w).reshape(NEXP, 8, 128, 4 * 512)
    hc = host_consts()
    for k in hc:
        if k in need:
            sh[k] = hc[k]
    return sh


def host_core(inp, core, need):
    m = {}
    t0 = core * TOK - HALO
    if "xw" in need:
        x = inp["x"][0]
        xw = np.zeros((WIN, D), np.float32)
        lo = max(t0, 0)
        xw[lo - t0:] = x[lo:t0 + WIN]
        m["xw"] = xw
    if "keybias" in need:
        pos = t0 + np.arange(WIN)
        kb = np.where(pos >= 0, 0.0, NEG).astype(np.float32)
        m["keybias"] = np.ascontiguousarray(kb.reshape(NWT, 128).T)
    if "cmask" in need:
        m["cmask"] = np.full((128, 2), 1.0 if core > 0 else 0.0, np.float32)
    return m


_CACHE = {}


def kernel(**inputs):
    if "nc" not in _CACHE:
        _CACHE["nc"] = build()
    nc, need = _CACHE["nc"]
    sh = host_shared(inputs, need)
    in_maps = []
    for c in range(NCORES):
        m = dict(sh)
        m.update(host_core(inputs, c, need))
        in_maps.append(m)
    res = run_bass_kernel_spmd(nc, in_maps, core_ids=list(range(NCORES)))
    return np.concatenate([np.asarray(r["out"]) for r in res.results], axis=0).reshape(1, S, D).astype(np.float32)
```
